# Optimizing a Trainium2 kernel written in Bass

```python
import math
import jax, jax.numpy as jnp
from jax import lax
import numpy as np

D_MODEL = 1024
BATCH = 8
SEQ = 2048
DEPTH = 2

N_MIXERS = 2
N_CONV_LAYERS = (DEPTH + 1) // 2
N_ATTN_LAYERS = DEPTH // 2
CONV_WIDTH = 31
N_HEADS = 8
HEAD_DIM = D_MODEL // N_HEADS
MOBA_BLOCK = 256
MOBA_TOPK = 3
Q_CHUNK = 64
N_GROUPS = 4
EXPERTS_PER_GROUP = 4
N_EXPERTS = N_GROUPS * EXPERTS_PER_GROUP
EXPERT_HIDDEN = 512
EXPERT_TOPK = 2
PLE_DIM = 256
EPS = 1e-6
NEG_INF = -1e30

kernel_name = 'hybrid_conformer_moba_hmoe'


def rms_norm(x, g):
    x32 = x.astype(jnp.float32)
    y = x32 * lax.rsqrt(jnp.mean(x32 * x32, axis=-1, keepdims=True) + EPS)
    return (y * g.astype(jnp.float32)).astype(x.dtype)


def layer_norm(x, g, b):
    x32 = x.astype(jnp.float32)
    mu = jnp.mean(x32, axis=-1, keepdims=True)
    xc = x32 - mu
    y = xc * lax.rsqrt(jnp.mean(xc * xc, axis=-1, keepdims=True) + EPS)
    return (y * g.astype(jnp.float32) + b.astype(jnp.float32)).astype(x.dtype)


def conformer_conv(xn, w_pw1, b_pw1, w_dw, b_dw, ln_g, ln_b, w_pw2, b_pw2):
    u = xn @ w_pw1 + b_pw1
    a, g = jnp.split(u, 2, axis=-1)
    u = a * jax.nn.sigmoid(g)
    u = lax.conv_general_dilated(
        u, w_dw[:, None, :], window_strides=(1,),
        padding=((CONV_WIDTH - 1, 0),),
        dimension_numbers=('NWC', 'WIO', 'NWC'),
        feature_group_count=D_MODEL) + b_dw
    u = jax.nn.silu(layer_norm(u, ln_g, ln_b))
    return u @ w_pw2 + b_pw2


def alibi_slopes():
    return jnp.exp2(-8.0 * jnp.arange(1, N_HEADS + 1, dtype=jnp.float32) / N_HEADS)


def moba_attention(xn, w_qkv, q_gain, k_gain, w_o):
    B, S, _ = xn.shape
    n_blk = -(-S // MOBA_BLOCK)
    s_pad = n_blk * MOBA_BLOCK
    n_qc = s_pad // Q_CHUNK
    k_sel_n = max(1, min(MOBA_TOPK, n_blk - 1))

    qkv = (xn @ w_qkv).reshape(B, S, 3, N_HEADS, HEAD_DIM)
    qkv = jnp.pad(qkv, ((0, 0), (0, s_pad - S), (0, 0), (0, 0), (0, 0)))
    q = rms_norm(qkv[:, :, 0], q_gain).transpose(0, 2, 1, 3)
    k = rms_norm(qkv[:, :, 1], k_gain).transpose(0, 2, 1, 3)
    v = qkv[:, :, 2].transpose(0, 2, 1, 3)
    k_blocks = k.reshape(B, N_HEADS, n_blk, MOBA_BLOCK, HEAD_DIM)
    v_blocks = v.reshape(B, N_HEADS, n_blk, MOBA_BLOCK, HEAD_DIM)

    q_blk = jnp.arange(s_pad) // MOBA_BLOCK
    k_mean = jnp.mean(k_blocks.astype(jnp.float32), axis=3)
    gate = jnp.einsum('bhsd,bhnd->bhsn', q.astype(jnp.float32), k_mean)
    fully_past = jnp.arange(n_blk)[None, :] < q_blk[:, None]
    gate = jnp.where(fully_past, gate, NEG_INF)
    _, sel_idx = lax.top_k(gate, k_sel_n)

    slopes = alibi_slopes()
    scale = HEAD_DIM ** -0.5
    h_ar = jnp.arange(N_HEADS)[:, None, None]

    def attend(step):
        b = step // n_qc
        start = (step % n_qc) * Q_CHUNK
        q_c = lax.dynamic_slice_in_dim(lax.dynamic_index_in_dim(q, b, 0, keepdims=False), start, Q_CHUNK, axis=1)
        kb = lax.dynamic_index_in_dim(k_blocks, b, 0, keepdims=False)
        vb = lax.dynamic_index_in_dim(v_blocks, b, 0, keepdims=False)
        idx = lax.dynamic_slice_in_dim(lax.dynamic_index_in_dim(sel_idx, b, 0, keepdims=False), start, Q_CHUNK, axis=1)
        t = start + jnp.arange(Q_CHUNK)
        j = start // MOBA_BLOCK
        k_own = lax.dynamic_index_in_dim(kb, j, 1, keepdims=False)
        v_own = lax.dynamic_index_in_dim(vb, j, 1, keepdims=False)
        dist_own = t[:, None] - (j * MOBA_BLOCK + jnp.arange(MOBA_BLOCK))[None, :]
        s_own = (jnp.einsum('hqd,hkd->hqk', q_c, k_own).astype(jnp.float32) * scale
                 - slopes[:, None, None] * dist_own.astype(jnp.float32))
        s_own = jnp.where(dist_own >= 0, s_own, NEG_INF)
        k_sel = kb[h_ar, idx]
        v_sel = vb[h_ar, idx]
        key_sel = idx[..., None] * MOBA_BLOCK + jnp.arange(MOBA_BLOCK)
        dist_sel = t[None, :, None, None] - key_sel
        s_sel = (jnp.einsum('hqd,hqnkd->hqnk', q_c, k_sel).astype(jnp.float32) * scale
                 - slopes[:, None, None, None] * dist_sel.astype(jnp.float32))
        valid = (jnp.arange(k_sel_n) < j)[None, None, :, None]
        s_sel = jnp.where(valid, s_sel, NEG_INF)
        logits = jnp.concatenate([s_sel.reshape(N_HEADS, Q_CHUNK, k_sel_n * MOBA_BLOCK), s_own], axis=-1)
        probs = jax.nn.softmax(logits, axis=-1).astype(v.dtype)
        p_sel = probs[..., :k_sel_n * MOBA_BLOCK].reshape(N_HEADS, Q_CHUNK, k_sel_n, MOBA_BLOCK)
        p_own = probs[..., k_sel_n * MOBA_BLOCK:]
        return (jnp.einsum('hqnk,hqnkd->hqd', p_sel, v_sel)
                + jnp.einsum('hqk,hkd->hqd', p_own, v_own))

    outs = lax.map(attend, jnp.arange(B * n_qc))
    o = outs.reshape(B, n_qc, N_HEADS, Q_CHUNK, HEAD_DIM).transpose(0, 1, 3, 2, 4)
    o = o.reshape(B, s_pad, D_MODEL)[:, :S]
    return o @ w_o


def hier_moe(xn, w_group, b_group, w_router, b_router, w_gate, w_up, w_down):
    B, S, D = xn.shape
    xf = xn.reshape(-1, D)
    g_prob = jax.nn.softmax((xf @ w_group + b_group).astype(jnp.float32), axis=-1)
    g_w, g_idx = lax.top_k(g_prob, 1)
    e_logits = jnp.einsum('td,gde->tge', xf, w_router) + b_router
    e_logits = jnp.take_along_axis(e_logits, g_idx[:, :, None], axis=1)[:, 0].astype(jnp.float32)
    e_prob = jax.nn.softmax(e_logits, axis=-1)
    e_w, e_idx = lax.top_k(e_prob, EXPERT_TOPK)
    e_w = e_w / jnp.sum(e_w, axis=-1, keepdims=True)
    weights = g_w * e_w
    expert_id = g_idx * EXPERTS_PER_GROUP + e_idx
    combine = jnp.sum(jax.nn.one_hot(expert_id, N_EXPERTS, dtype=jnp.float32) * weights[..., None],
                      axis=1).astype(xn.dtype)
    y = jnp.zeros_like(xf)
    for e in range(N_EXPERTS):
        hdn = jax.nn.silu(xf @ w_gate[e]) * (xf @ w_up[e])
        y = y + combine[:, e:e + 1] * (hdn @ w_down[e])
    return y.reshape(B, S, D)


def setup_inputs(seed: int = 0) -> dict:
    key = jax.random.key(seed)
    ks = jax.random.split(key, 26)

    def nrm(k, shape, scale):
        return jax.random.normal(k, shape, jnp.float32) * scale

    def gain(k, shape):
        return 1.0 + 0.05 * jax.random.normal(k, shape, jnp.float32)

    D = D_MODEL
    return {
        'x': nrm(ks[0], (BATCH, SEQ, D), 1.0),
        'p': nrm(ks[1], (DEPTH, BATCH, SEQ, PLE_DIM), 1.0),
        'g_mix': gain(ks[2], (DEPTH, D)),
        'g_ffn': gain(ks[3], (DEPTH, D)),
        'g_ple': gain(ks[4], (DEPTH, D)),
        'conv_w_pw1': nrm(ks[5], (N_CONV_LAYERS, D, 2 * D), D ** -0.5),
        'conv_b_pw1': nrm(ks[6], (N_CONV_LAYERS, 2 * D), 0.02),
        'conv_w_dw': nrm(ks[7], (N_CONV_LAYERS, CONV_WIDTH, D), CONV_WIDTH ** -0.5),
        'conv_b_dw': nrm(ks[8], (N_CONV_LAYERS, D), 0.02),
        'conv_ln_g': gain(ks[9], (N_CONV_LAYERS, D)),
        'conv_ln_b': nrm(ks[10], (N_CONV_LAYERS, D), 0.02),
        'conv_w_pw2': nrm(ks[11], (N_CONV_LAYERS, D, D), D ** -0.5),
        'conv_b_pw2': nrm(ks[12], (N_CONV_LAYERS, D), 0.02),
        'attn_w_qkv': nrm(ks[13], (N_ATTN_LAYERS, D, 3 * D), D ** -0.5),
        'attn_q_gain': gain(ks[14], (N_ATTN_LAYERS, HEAD_DIM)),
        'attn_k_gain': gain(ks[15], (N_ATTN_LAYERS, HEAD_DIM)),
        'attn_w_o': nrm(ks[16], (N_ATTN_LAYERS, D, D), D ** -0.5),
        'moe_w_group': nrm(ks[17], (DEPTH, D, N_GROUPS), D ** -0.5),
        'moe_b_group': nrm(ks[18], (DEPTH, N_GROUPS), 0.01),
        'moe_w_router': nrm(ks[19], (DEPTH, N_GROUPS, D, EXPERTS_PER_GROUP), D ** -0.5),
        'moe_b_router': nrm(ks[20], (DEPTH, N_GROUPS, EXPERTS_PER_GROUP), 0.01),
        'moe_w_gate': nrm(ks[21], (DEPTH, N_EXPERTS, D, EXPERT_HIDDEN), D ** -0.5),
        'moe_w_up': nrm(ks[22], (DEPTH, N_EXPERTS, D, EXPERT_HIDDEN), D ** -0.5),
        'moe_w_down': nrm(ks[23], (DEPTH, N_EXPERTS, EXPERT_HIDDEN, D), EXPERT_HIDDEN ** -0.5),
        'ple_w_gate': nrm(ks[24], (DEPTH, D, D), D ** -0.5),
        'ple_w_proj': nrm(ks[25], (DEPTH, PLE_DIM, D), PLE_DIM ** -0.5),
    }


def reference(x, p, g_mix, g_ffn, g_ple,
              conv_w_pw1, conv_b_pw1, conv_w_dw, conv_b_dw, conv_ln_g, conv_ln_b, conv_w_pw2, conv_b_pw2,
              attn_w_qkv, attn_q_gain, attn_k_gain, attn_w_o,
              moe_w_group, moe_b_group, moe_w_router, moe_b_router, moe_w_gate, moe_w_up, moe_w_down,
              ple_w_gate, ple_w_proj):
    h = x
    for i in range(DEPTH):
        xn = rms_norm(h, g_mix[i])
        if i % N_MIXERS == 0:
            c = i // N_MIXERS
            mix = conformer_conv(xn, conv_w_pw1[c], conv_b_pw1[c], conv_w_dw[c], conv_b_dw[c],
                                 conv_ln_g[c], conv_ln_b[c], conv_w_pw2[c], conv_b_pw2[c])
        else:
            a = i // N_MIXERS
            mix = moba_attention(xn, attn_w_qkv[a], attn_q_gain[a], attn_k_gain[a], attn_w_o[a])
        h = h + mix
        h = h + hier_moe(rms_norm(h, g_ffn[i]), moe_w_group[i], moe_b_group[i], moe_w_router[i],
                         moe_b_router[i], moe_w_gate[i], moe_w_up[i], moe_w_down[i])
        ple_gate = jax.nn.sigmoid(rms_norm(h, g_ple[i]) @ ple_w_gate[i])
        h = h + ple_gate * (p[i] @ ple_w_proj[i])
    return h
```

```python
import math
from contextlib import ExitStack

import numpy as np
import concourse.bass as bass
import concourse.mybir as mybir
from concourse.bass_utils import run_bass_kernel_spmd

F32 = mybir.dt.float32
BF16 = mybir.dt.bfloat16
AF = mybir.ActivationFunctionType
ALU = mybir.AluOpType
AX = mybir.AxisListType

D = 1024
S_LEN = 2048
NC8 = 8
NTT = 4
TT = 512
HALO = 30
NH = 8
HD = 128
NE = 16
EH = 512
PLE = 256
EPS = 1e-6
NCORES = 8
DBG_BARRIER = False


class Sched:
    def __init__(self, nc, ctx):
        self.nc = nc
        self.ctx = ctx
        self.eng = {'pe': nc.tensor, 'act': nc.scalar, 'dve': nc.vector, 'pool': nc.gpsimd, 'sp': nc.sync}
        self.sem = {}
        self.cnt = {}
        self.nsem = 0
        for e in ['pe', 'act', 'dve', 'pool']:
            self._new_sem(e)
        self.seen = {e: {} for e in self.eng}
        self.W = {}
        self.R = {}
        self.pend = {e: [] for e in self.eng}
        self.dma_sems = []
        self.dma_cnt = []
        self.dma_rr = {'hw': 0, 'sw': 0}
        self.dma_pool = {'hw': list(range(0, 8)), 'sw': list(range(8, 32))}
        for i in range(32):
            self.dma_sems.append(ctx.enter_context(nc.semaphore(f"dq{i}")))
            self.dma_cnt.append(0)
        self.nwaits = 0
        self.nops = 0

    def _new_sem(self, e):
        self.nsem += 1
        self.sem[e] = self.ctx.enter_context(self.nc.semaphore(f"s_{e}_{self.nsem}"))
        self.cnt[e] = 0

    @staticmethod
    def regions(ap):
        t = ap.tensor
        if str(ap.space) == 'DRAM':
            return []
        shape = t.shape
        row = 1
        for s in shape[1:]:
            row *= s
        off = int(ap.offset)
        dims = ap.ap
        p0 = off // row
        f0 = off % row
        npart = dims[0][1]
        fd = sorted([(abs(s), n) for (s, n) in dims[1:] if n > 1 and s != 0], reverse=True)

        def expand(ds, base, budget):
            if not ds:
                return [(base, base + 1)]
            s0, n0 = ds[0]
            inner = 1
            for (t_, m_) in ds[1:]:
                inner += (m_ - 1) * t_
            if s0 > inner and n0 <= budget:
                out = []
                for i in range(n0):
                    out += expand(ds[1:], base + i * s0, budget // n0)
                return out
            return [(base, base + inner + (n0 - 1) * s0)]

        name = t.name
        return [(name, p0, p0 + npart, a, b) for (a, b) in expand(fd, f0, 32)]

    @staticmethod
    def _ov(a, r):
        return a[0] < r[2] and r[1] < a[1] and a[2] < r[4] and r[3] < a[3]

    def _collect(self, engine, ins, outs):
        toks = []
        rin = [r for a in ins for r in self.regions(a)]
        rout = [r for a in outs for r in self.regions(a)]
        same = engine == 'pe'
        for r in rin:
            if r is None:
                continue
            for w in self.W.get(r[0], ()):
                if self._ov(w, r):
                    toks.append(w[4])
        for r in rout:
            if r is None:
                continue
            for w in self.W.get(r[0], ()):
                if self._ov(w, r) and not (same and w[4][2] == engine):
                    toks.append(w[4])
            for rd in self.R.get(r[0], ()):
                if self._ov(rd, r) and not (same and rd[4][2] == engine):
                    toks.append(rd[4])
        return toks, rin, rout

    def _wait(self, engine, toks):
        need = {}
        for (sem, val, src) in toks:
            k = id(sem)
            if k not in need or need[k][1] < val:
                need[k] = (sem, val, src)
        for k, (sem, val, src) in need.items():
            if src == engine and engine == 'pe':
                continue
            if self.seen[engine].get(k, 0) < val:
                self.eng[engine].wait_ge(sem, val)
                self.seen[engine][k] = val
                self.nwaits += 1

    def _record(self, tok, rin, rout):
        for r in rin:
            if r is None:
                continue
            lst = self.R.setdefault(r[0], [])
            lst[:] = [x for x in lst if not (x[4][0] is tok[0] and x[0] >= r[1] and x[1] <= r[2]
                                             and x[2] >= r[3] and x[3] <= r[4])]
            lst.append((r[1], r[2], r[3], r[4], tok))
        for r in rout:
            if r is None:
                continue
            lst = self.W.setdefault(r[0], [])
            lst[:] = [x for x in lst if not (x[0] >= r[1] and x[1] <= r[2] and x[2] >= r[3] and x[3] <= r[4])]
            lst.append((r[1], r[2], r[3], r[4], tok))
            rl = self.R.get(r[0])
            if rl:
                rl[:] = [x for x in rl if not (x[0] >= r[1] and x[1] <= r[2] and x[2] >= r[3] and x[3] <= r[4])]

    def op(self, engine, fn, ins=(), outs=(), inc=True):
        toks, rin, rout = self._collect(engine, ins, outs)
        self._wait(engine, toks)
        ins_obj = fn(self.eng[engine])
        self.nops += 1
        if inc:
            if self.cnt[engine] >= 30000:
                self._new_sem(engine)
            self.cnt[engine] += 1
            ins_obj.then_inc(self.sem[engine], 1)
            tok = (self.sem[engine], self.cnt[engine], engine)
            for (pi, po) in self.pend[engine]:
                self._record(tok, pi, po)
            self.pend[engine] = []
            self._record(tok, rin, rout)
            return tok
        else:
            self.pend[engine].append((rin, rout))
            return None

    def dma(self, queue, out, in_, **kw):
        kind = 'sw' if queue == 'pool' else 'hw'
        lst = self.dma_pool[kind]
        i = lst[self.dma_rr[kind] % len(lst)]
        self.dma_rr[kind] += 1
        sem = self.dma_sems[i]
        toks, rin, rout = self._collect('dma', [in_], [out])
        if self.dma_cnt[i] > 0:
            toks.append((sem, self.dma_cnt[i], 'dma'))
        self._wait(queue, toks)
        ins_obj = self.eng[queue].dma_start(out=out, in_=in_, **kw)
        self.dma_cnt[i] += 16
        ins_obj.then_inc(sem, 16)
        tok = (sem, self.dma_cnt[i], 'dma')
        self._record(tok, rin, rout)
        return tok

    def wait_tok(self, engine, tok):
        self._wait(engine, [tok])

    def mm(self, out, lhsT, rhs, start, stop, inc=None):
        inc = True
        return self.op('pe', lambda e: e.matmul(out, lhsT=lhsT, rhs=rhs, start=start, stop=stop),
                       ins=[lhsT, rhs], outs=[out], inc=inc)

    def transpose(self, out, in_, ident):
        return self.op('pe', lambda e: e.transpose(out, in_, ident), ins=[in_, ident], outs=[out])

    def act(self, out, in_, func, bias=None, scale=None, eng='act'):
        ins = [in_]
        kw = {}
        if bias is not None:
            kw['bias'] = bias
            if not isinstance(bias, (int, float)):
                ins.append(bias)
        if scale is not None:
            kw['scale'] = scale
            if not isinstance(scale, (int, float)):
                ins.append(scale)
        return self.op('act', lambda e: e.activation(out=out, in_=in_, func=func, **kw), ins=ins, outs=[out])

    def tt(self, eng, out, in0, in1, op):
        return self.op(eng, lambda e: e.tensor_tensor(out=out, in0=in0, in1=in1, op=op), ins=[in0, in1], outs=[out])

    def ts(self, eng, out, in0, s1, op0, s2=None, op1=None):
        ins = [in0]
        if not isinstance(s1, (int, float)):
            ins.append(s1)
        if s2 is not None and not isinstance(s2, (int, float)):
            ins.append(s2)
        if op1 is None:
            return self.op(eng, lambda e: e.tensor_scalar(out=out, in0=in0, scalar1=s1, scalar2=None, op0=op0),
                           ins=ins, outs=[out])
        return self.op(eng, lambda e: e.tensor_scalar(out=out, in0=in0, scalar1=s1, scalar2=s2, op0=op0, op1=op1),
                       ins=ins, outs=[out])

    def stt(self, out, in0, scalar, in1, op0, op1):
        ins = [in0, in1]
        if not isinstance(scalar, (int, float)):
            ins.append(scalar)
        return self.op('dve', lambda e: e.scalar_tensor_tensor(out=out, in0=in0, scalar=scalar, in1=in1, op0=op0, op1=op1),
                       ins=ins, outs=[out])

    def copy(self, eng, out, in_):
        if eng == 'act':
            return self.op('act', lambda e: e.copy(out=out, in_=in_), ins=[in_], outs=[out])
        return self.op(eng, lambda e: e.tensor_copy(out=out, in_=in_), ins=[in_], outs=[out])

    def recip(self, out, in_):
        return self.op('dve', lambda e: e.reciprocal(out=out, in_=in_), ins=[in_], outs=[out])

    def recip_fast(self, out, in_):
        return self.op('dve', lambda e: e.reciprocal_approx_fast(out=out, in_=in_), ins=[in_], outs=[out])

    def reduce(self, out, in_, op):
        return self.op('dve', lambda e: e.tensor_reduce(out=out, in_=in_, axis=AX.X, op=op), ins=[in_], outs=[out])

    def memset(self, eng, ap, val):
        return self.op(eng, lambda e: e.memset(ap, val), ins=[], outs=[ap])


class Packer:
    def __init__(self):
        self.cols = []
        self.off = {}
        self.n = 0

    def add(self, name, arr):
        arr = np.ascontiguousarray(arr, dtype=np.float32)
        assert arr.shape[0] == 128, (name, arr.shape)
        arr = arr.reshape(128, -1)
        self.off[name] = self.n
        self.n += arr.shape[1]
        self.cols.append(arr)

    def build(self):
        return np.ascontiguousarray(np.concatenate(self.cols, axis=1))


def feat_cols(v):
    v = np.asarray(v, dtype=np.float32)
    return v.reshape(-1, 128).T


def prm_layout():
    off = {}
    n = 0
    for name, w in [('g_mix', 16), ('g_ffn', 16), ('g_ple', 16), ('b_pw1', 16), ('w_dw', 248), ('b_dw', 8),
                    ('ln_g', 8), ('ln_b', 8), ('b_pw2', 8), ('q_gain', 1), ('k_gain', 1),
                    ('wr0', 160), ('wr1', 160), ('rb0', 20), ('rb1', 20)]:
        off[name] = n
        n += w
    return off, n


def pack_params(inp):
    pk = Packer()
    pk.add('g_mix', feat_cols(inp['g_mix']))
    pk.add('g_ffn', feat_cols(inp['g_ffn']))
    pk.add('g_ple', feat_cols(inp['g_ple']))
    pk.add('b_pw1', feat_cols(inp['conv_b_pw1'][0]))
    wdw = np.asarray(inp['conv_w_dw'][0], dtype=np.float32)
    pk.add('w_dw', wdw.reshape(31, 8, 128).transpose(2, 1, 0).reshape(128, 248))
    pk.add('b_dw', feat_cols(inp['conv_b_dw'][0]))
    pk.add('ln_g', feat_cols(inp['conv_ln_g'][0]))
    pk.add('ln_b', feat_cols(inp['conv_ln_b'][0]))
    pk.add('b_pw2', feat_cols(inp['conv_b_pw2'][0]))
    pk.add('q_gain', np.asarray(inp['attn_q_gain'][0], dtype=np.float32).reshape(128, 1))
    pk.add('k_gain', np.asarray(inp['attn_k_gain'][0], dtype=np.float32).reshape(128, 1))
    for l in range(2):
        wg = np.asarray(inp['moe_w_group'][l], dtype=np.float32)
        wr = np.asarray(inp['moe_w_router'][l], dtype=np.float32)
        wall = np.concatenate([wg, wr.transpose(1, 0, 2).reshape(1024, 16)], axis=1)
        pk.add(f'wr{l}', wall.reshape(8, 128, 20).transpose(1, 0, 2).reshape(128, 160))
    for l in range(2):
        b = np.concatenate([np.asarray(inp['moe_b_group'][l], dtype=np.float32).reshape(4),
                            np.asarray(inp['moe_b_router'][l], dtype=np.float32).reshape(16)])
        pk.add(f'rb{l}', np.tile(b[None, :], (128, 1)))
    arr = pk.build()
    off, n = prm_layout()
    assert off == pk.off and n == arr.shape[1]
    return arr


def cst_layout():
    off = {}
    n = 0
    for name, w in [('ident', 128), ('tri', 128), ('ab', 8 * 17), ('alc', 16), ('negm', 64), ('esel', 9 * 128)]:
        off[name] = n
        n += w
    return off, n


def make_consts():
    pk = Packer()
    pk.add('ident', np.eye(128, dtype=np.float32))
    p = np.arange(128)[:, None]
    f = np.arange(128)[None, :]
    pk.add('tri', (f >= p).astype(np.float32))
    slopes = np.array([2.0 ** (-(i + 1)) for i in range(8)], dtype=np.float64)
    ab = np.zeros((128, 8, 17), dtype=np.float64)
    for h in range(8):
        for d in range(17):
            ab[:, h, d] = slopes[h] * (np.arange(128) - 128.0 * (d - 1))
    pk.add('ab', ab.reshape(128, 136))
    alc = np.zeros((128, 8, 2), dtype=np.float64)
    for h in range(8):
        for par in range(2):
            alc[:, h, par] = -slopes[h] * (par * 128 + np.arange(128))
    pk.add('alc', alc.reshape(128, 16))
    negm = np.zeros((128, 8, 8), dtype=np.float32)
    for i8 in range(8):
        j = (8 + i8) // 2
        for n in range(8):
            if n >= j:
                negm[:, i8, n] = -1e30
    pk.add('negm', negm.reshape(128, 64))
    esel = np.zeros((128, 9, 128), dtype=np.float32)
    for s in range(9):
        esel[0, s, :] = 1.0
        if s < 8:
            esel[32 + s, s, :] = 1.0
    pk.add('esel', esel.reshape(128, 9 * 128))
    arr = pk.build()
    off, n = cst_layout()
    assert off == pk.off and n == arr.shape[1]
    return arr


def build_program(phases=('A', 'B0', 'C0', 'D', 'B1', 'C1'), debug=False):
    nc = bass.Bass("TRN2", target_bir_lowering=False)
    PO, NPRM = prm_layout()
    CO, NCST = cst_layout()

    def din(name, shape):
        return nc.dram_tensor(name, list(shape), F32, kind="ExternalInput").ap()

    xT = din("xT", [D, S_LEN])
    pT = din("pT", [2, PLE, S_LEN])
    prm_d = din("prm", [128, NPRM])
    cst_d = din("cst", [128, NCST])
    w_pw1 = din("w_pw1", [D, 2 * D])
    w_pw2 = din("w_pw2", [D, D])
    w_qkv = din("w_qkv", [D, 3 * D])
    w_o = din("w_o", [D, D])
    w_gate = din("w_gate", [2, NE, D, EH])
    w_up = din("w_up", [2, NE, D, EH])
    w_down = din("w_down", [2, NE, EH, D])
    w_pg = din("w_pg", [2, D, D])
    w_pp = din("w_pp", [2, PLE, D])
    yT = nc.dram_tensor("yT", [D, S_LEN], F32, kind="ExternalOutput").ap()
    dbg = nc.dram_tensor("dbg", [128, 8192], F32, kind="ExternalOutput").ap() if debug else None

    with ExitStack() as ctx:
        S = Sched(nc, ctx)

        def sb(name, shape, dt, c=ctx):
            return c.enter_context(nc.sbuf_tensor(name, list(shape), dt))

        h = sb("h", [128, NC8, S_LEN], F32)
        xg = sb("xg", [128, NC8, HALO + S_LEN], BF16)
        prm = sb("prm_s", [128, NPRM], F32)
        cst = sb("cst_s", [128, NCST], F32)
        ones_bf = sb("ones_bf", [128, 128], BF16)
        tri_bf = sb("tri_bf", [128, 128], BF16)
        esel_bf = sb("esel_bf", [128, 9, 128], BF16)
        sq = sb("sq", [128, 2, TT], BF16)
        rt = sb("rt", [128, TT], F32)
        rstd = sb("rstd", [128, TT], F32)
        PS = [ctx.enter_context(nc.psum_tensor(f"ps{i}", [128, 512], F32)) for i in range(8)]
        ps_rr = [0]
        ps_set = [list(range(8))]

        def next_ps():
            lst = ps_set[0]
            b = lst[ps_rr[0] % len(lst)]
            ps_rr[0] += 1
            return PS[b]

        ident = cst[:, CO['ident']:CO['ident'] + 128]

        def xn(c, lo, n):
            return xg[:, c, HALO + lo:HALO + lo + n]

        def P(name, j=0, n=1):
            return prm[:, PO[name] + j:PO[name] + j + n]

        S.dma('sp', cst[:], cst_d)
        S.dma('sp', prm[:], prm_d)
        xv = xT.rearrange("(c p) t -> p c t", p=128)
        for c in range(NC8):
            S.dma('sp' if c % 2 == 0 else 'act', h[:, c, :], xv[:, c, :])
        S.memset('dve', ones_bf[:], 1.0)
        S.copy('dve', tri_bf[:], cst[:, CO['tri']:CO['tri'] + 128])
        S.copy('dve', esel_bf[:].rearrange("p a b -> p (a b)"), cst[:, CO['esel']:CO['esel'] + 9 * 128])
        S.memset('pool', xg[:, :, 0:HALO], 0.0)

        def tts(tt):
            return slice(tt * TT, (tt + 1) * TT)

        dbg_toks = []

        def dump(ap, col):
            if dbg is None:
                return
            np_, n = ap.shape[0], ap.shape[1]
            dbg_toks.append(S.dma('sp', dbg[0:np_, col:col + n], ap))

        def rms_tile(tt, gname, gl, out_fn):
            ps = next_ps()
            for c in range(NC8):
                S.act(sq[:, c % 2, :], h[:, c, tts(tt)], AF.Square)
                S.mm(ps[:], ones_bf[:], sq[:, c % 2, :], start=(c == 0), stop=(c == NC8 - 1))
            S.act(rt[:], ps[:], AF.Ln, bias=EPS, scale=1.0 / D)
            S.act(rstd[:], rt[:], AF.Exp, scale=-0.5)
            for c in range(NC8):
                S.stt(out_fn(c), h[:, c, tts(tt)], P(gname, gl * 8 + c), rstd[:], ALU.mult, ALU.mult)

        def barrier():
            toks = []
            for e in ['pe', 'act', 'dve', 'pool']:
                if S.cnt[e] > 0:
                    toks.append((S.sem[e], S.cnt[e], e))
            for i, sem in enumerate(S.dma_sems):
                if S.dma_cnt[i] > 0:
                    toks.append((sem, S.dma_cnt[i], 'dma'))
            for e in ['pe', 'act', 'dve', 'pool', 'sp']:
                S._wait(e, toks)

        def phase_A():
            with ExitStack() as pc:
                rms_tile(0, 'g_mix', 0, lambda c: xn(c, 0, TT))
                glu = sb("glu", [128, NC8, HALO + S_LEN], BF16, pc)
                S.memset('pool', glu[:, :, 0:HALO], 0.0)
                with ExitStack() as p2:
                    w1 = sb("w1", [128, NC8, 2 * D], BF16, p2)
                    sg = sb("sgA", [128, 2, TT], F32, p2)
                    w1v = w_pw1.rearrange("(c p) n -> p c n", p=128)
                    for c in range(NC8):
                        S.dma('pool', w1[:, c, :], w1v[:, c, :])
                    for tt in range(NTT):
                        if tt + 1 < NTT:
                            rms_tile(tt + 1, 'g_mix', 0, lambda c, t1=tt + 1: xn(c, t1 * TT, TT))
                        for c in range(NC8):
                            pa = next_ps()
                            pg = next_ps()
                            for k in range(NC8):
                                S.mm(pa[:], w1[:, k, c * 128:(c + 1) * 128], xn(k, tt * TT, TT), k == 0, k == NC8 - 1)
                            for k in range(NC8):
                                S.mm(pg[:], w1[:, k, D + c * 128:D + (c + 1) * 128], xn(k, tt * TT, TT), k == 0, k == NC8 - 1)
                            S.act(sg[:, c % 2, :], pg[:], AF.Sigmoid, bias=P('b_pw1', 8 + c))
                            S.stt(glu[:, c, HALO + tt * TT:HALO + (tt + 1) * TT], pa[:], P('b_pw1', c), sg[:, c % 2, :],
                                  ALU.add, ALU.mult)
                    barrier()
                dg = sb("dg", [128, 2, 31, 128], BF16, pc)
                w2 = sb("w2", [128, NC8, D], BF16, pc)
                w2v = w_pw2.rearrange("(c p) n -> p c n", p=128)
                for c in range(NC8):
                    S.dma('pool', w2[:, c, :], w2v[:, c, :])

                def cvb(c, tt):
                    return xn(c, tt * TT, TT)

                for c in range(NC8):
                    wd = prm[:, PO['w_dw'] + c * 31:PO['w_dw'] + (c + 1) * 31]
                    S.tt('dve', dg[:, c % 2], ident.unsqueeze(1).to_broadcast([128, 31, 128]),
                         wd.unsqueeze(2).to_broadcast([128, 31, 128]), ALU.mult)
                    for tt in range(NTT):
                        ps = next_ps()
                        for j in range(31):
                            S.mm(ps[:], dg[:, c % 2, j, :], glu[:, c, tt * TT + j:tt * TT + j + TT], j == 0, j == 30)
                        S.act(cvb(c, tt), ps[:], AF.Identity, bias=P('b_dw', c))
                mean = sb("meanA", [128, TT], F32, pc)
                m2 = sb("m2A", [128, TT], F32, pc)
                var = sb("varA", [128, TT], F32, pc)
                t1 = sb("t1A", [128, 2, TT], F32, pc)
                t2 = sb("t2A", [128, 2, TT], F32, pc)
                for tt in range(NTT):
                    p1 = next_ps()
                    p2_ = next_ps()
                    for c in range(NC8):
                        S.act(sq[:, c % 2, :], cvb(c, tt), AF.Square)
                        S.mm(p2_[:], ones_bf[:], sq[:, c % 2, :], c == 0, c == NC8 - 1)
                    for c in range(NC8):
                        S.mm(p1[:], ones_bf[:], cvb(c, tt), c == 0, c == NC8 - 1)
                    S.ts('dve', mean[:], p1[:], 1.0 / D, ALU.mult)
                    S.tt('dve', m2[:], mean[:], mean[:], ALU.mult)
                    S.stt(var[:], p2_[:], 1.0 / D, m2[:], ALU.mult, ALU.subtract)
                    S.act(rt[:], var[:], AF.Ln, bias=EPS)
                    S.act(rstd[:], rt[:], AF.Exp, scale=-0.5)
                    for c in range(NC8):
                        S.tt('pool', t1[:, c % 2, :], cvb(c, tt), mean[:], ALU.subtract)
                        S.tt('dve', t2[:, c % 2, :], t1[:, c % 2, :], rstd[:], ALU.mult)
                        S.act(xn(c, tt * TT, TT), t2[:, c % 2, :], AF.Silu, bias=P('ln_b', c), scale=P('ln_g', c))
                for tt in range(NTT):
                    for co in range(NC8):
                        ps = next_ps()
                        for k in range(NC8):
                            S.mm(ps[:], w2[:, k, co * 128:(co + 1) * 128], xn(k, tt * TT, TT), k == 0, k == NC8 - 1)
                        S.stt(h[:, co, tts(tt)], ps[:], P('b_pw2', co), h[:, co, tts(tt)], ALU.add, ALU.add)
            barrier()

        def phase_B(l):
            with ExitStack() as pc:
                cT_hi = sb(f"cThi_{l}", [128, S_LEN], BF16, pc)
                cT_lo = sb(f"cTlo_{l}", [128, S_LEN], BF16, pc)
                sel16 = sb(f"sel16_{l}", [128, 16, 128], BF16, pc)
                S.copy('dve', sel16[:], cst[:, CO['ident']:CO['ident'] + 16].unsqueeze(2).to_broadcast([128, 16, 128]))
                wr = prm[:, PO[f'wr{l}']:PO[f'wr{l}'] + 160]
                psL = PS[7]
                ps_set[0] = list(range(7))
                rc = ExitStack()
                combT = sb(f"combT_{l}", [128, S_LEN], F32, rc)
                S.memset('pool', combT[:], 0.0)
                x32 = sb(f"x32_{l}", [128, NC8, TT], F32, rc)
                lb = sb(f"lb_{l}", [128, 16, 20], F32, rc)
                lT = sb(f"lT_{l}", [128, 2, TT], F32, rc)
                for tt in range(NTT):
                    rms_tile(tt, 'g_ffn', l, lambda c: x32[:, c, :])
                    for c in range(NC8):
                        S.copy('act', xn(c, tt * TT, TT), x32[:, c, :])
                    pT_ = next_ps()
                    for c in range(NC8):
                        S.mm(pT_[0:20, :], wr[:, c * 20:(c + 1) * 20], x32[:, c, :], c == 0, c == NC8 - 1)
                    S.copy('act', lT[0:20, tt % 2, :], pT_[0:20, :])
                    for i4 in range(4):
                        i = tt * 4 + i4
                        S.transpose(psL[:, i * 20:(i + 1) * 20], lT[0:20, tt % 2, i4 * 128:(i4 + 1) * 128],
                                    cst[0:20, CO['ident']:CO['ident'] + 20])
                with rc:
                    def T(name, shape):
                        return sb(f"{name}_{l}", shape, F32, rc)
                    rb = prm[:, PO[f'rb{l}']:PO[f'rb{l}'] + 20]
                    S.tt('dve', lb[:], psL[:, 0:320].rearrange("p (t n) -> p t n", n=20),
                         rb.unsqueeze(1).to_broadcast([128, 16, 20]), ALU.add)
                    lg = lb[:, :, 0:4]
                    le = lb[:, :, 4:20].rearrange("p t (g e) -> p t g e", e=4)
                    m = T("r_m", [128, 16])
                    S.reduce(m[:], lg, ALU.max)
                    dd = T("r_d", [128, 16, 4])
                    S.tt('dve', dd[:], lg, m[:].unsqueeze(2).to_broadcast([128, 16, 4]), ALU.subtract)
                    oh = T("r_oh", [128, 16, 4])
                    S.ts('dve', oh[:], dd[:], 0.0, ALU.is_equal)
                    eg = T("r_eg", [128, 16, 4])
                    S.act(eg[:], dd[:], AF.Exp)
                    se = T("r_se", [128, 16])
                    S.reduce(se[:], eg[:], ALU.add)
                    gw = T("r_gw", [128, 16])
                    S.recip(gw[:], se[:])
                    tmp = T("r_tmp", [128, 16, 4, 4])
                    S.tt('dve', tmp[:], le, oh[:].unsqueeze(3).to_broadcast([128, 16, 4, 4]), ALU.mult)
                    sel = T("r_sel", [128, 16, 4])
                    S.reduce(sel[:], tmp[:].rearrange("p t g e -> p t e g"), ALU.add)
                    m1 = T("r_m1", [128, 16])
                    S.reduce(m1[:], sel[:], ALU.max)
                    eq1 = T("r_eq1", [128, 16, 4])
                    S.tt('dve', eq1[:], sel[:], m1[:].unsqueeze(2).to_broadcast([128, 16, 4]), ALU.is_equal)
                    sel2 = T("r_sel2", [128, 16, 4])
                    S.stt(sel2[:].rearrange("p t e -> p (t e)"), eq1[:].rearrange("p t e -> p (t e)"), -1e30,
                          sel[:].rearrange("p t e -> p (t e)"), ALU.mult, ALU.add)
                    m2_ = T("r_m2", [128, 16])
                    S.reduce(m2_[:], sel2[:], ALU.max)
                    eq2 = T("r_eq2", [128, 16, 4])
                    S.tt('dve', eq2[:], sel2[:], m2_[:].unsqueeze(2).to_broadcast([128, 16, 4]), ALU.is_equal)
                    d21 = T("r_d21", [128, 16])
                    S.tt('dve', d21[:], m2_[:], m1[:], ALU.subtract)
                    e21 = T("r_e21", [128, 16])
                    S.act(e21[:], d21[:], AF.Exp)
                    den = T("r_den", [128, 16])
                    S.ts('dve', den[:], e21[:], 1.0, ALU.add)
                    w1_ = T("r_w1", [128, 16])
                    S.recip(w1_[:], den[:])
                    w2_ = T("r_w2", [128, 16])
                    S.tt('dve', w2_[:], e21[:], w1_[:], ALU.mult)
                    w1g = T("r_w1g", [128, 16])
                    S.tt('dve', w1g[:], w1_[:], gw[:], ALU.mult)
                    w2g = T("r_w2g", [128, 16])
                    S.tt('dve', w2g[:], w2_[:], gw[:], ALU.mult)
                    ea = T("r_ea", [128, 16, 4])
                    S.tt('dve', ea[:], eq1[:], w1g[:].unsqueeze(2).to_broadcast([128, 16, 4]), ALU.mult)
                    eb = T("r_eb", [128, 16, 4])
                    S.tt('dve', eb[:], eq2[:], w2g[:].unsqueeze(2).to_broadcast([128, 16, 4]), ALU.mult)
                    ew = T("r_ew", [128, 16, 4])
                    S.tt('dve', ew[:], ea[:], eb[:], ALU.add)
                    comb = T("r_comb", [128, 16, 4, 4])
                    S.tt('dve', comb[:], oh[:].unsqueeze(3).to_broadcast([128, 16, 4, 4]),
                         ew[:].unsqueeze(2).to_broadcast([128, 16, 4, 4]), ALU.mult)
                    dump(lb[:].rearrange("p t n -> p (t n)"), 0)
                    dump(comb[:].rearrange("p t g e -> p (t g e)"), 320)
                    dump(oh[:].rearrange("p t g -> p (t g)"), 576)
                    dump(sel[:].rearrange("p t g -> p (t g)"), 640)
                    dump(ew[:].rearrange("p t g -> p (t g)"), 704)
                    dump(gw[:], 768)
                    for g4 in range(4):
                        ps = next_ps()
                        for i4 in range(4):
                            i = g4 * 4 + i4
                            S.transpose(ps[0:16, i4 * 128:(i4 + 1) * 128],
                                        comb[:, i, :, :].rearrange("p g e -> p (g e)"), ident)
                        S.copy('act', combT[0:16, g4 * 512:(g4 + 1) * 512], ps[0:16, :])
                    S.copy('dve', cT_hi[:], combT[:])
                    S.tt('dve', combT[:], combT[:], cT_hi[:], ALU.subtract)
                    S.copy('dve', cT_lo[:], combT[:])
                    barrier()
                    dump(combT[0:16, :], 1024)
                ps_set[0] = list(range(8))
                wg = sb(f"wg_{l}", [128, 2, NC8, EH], BF16, pc)
                wu = sb(f"wu_{l}", [128, 2, NC8, EH], BF16, pc)
                wd = sb(f"wd_{l}", [128, 2, 4, D], BF16, pc)
                sgs = sb(f"sgs_{l}", [128, 3, TT], F32, pc)
                tus = sb(f"tus_{l}", [128, 3, TT], F32, pc)
                hdn = sb(f"hdn_{l}", [128, 2, 4, TT], BF16, pc)

                def load_w(e):
                    b = e % 2
                    S.dma('pool', wg[:, b], w_gate[l, e].rearrange("(c p) n -> p c n", p=128))
                    S.dma('pool', wu[:, b], w_up[l, e].rearrange("(c p) n -> p c n", p=128))
                    S.dma('pool', wd[:, b], w_down[l, e].rearrange("(c p) n -> p c n", p=128))

                units = [(e, tt) for e in range(NE) for tt in range(NTT)]
                rr = [0]

                def gu(u, ui):
                    e, tt = u
                    b = e % 2
                    pcb = PS[6 + ui % 2]
                    S.mm(pcb[:], sel16[:, e, :], cT_hi[:, tts(tt)], True, False)
                    S.mm(pcb[:], sel16[:, e, :], cT_lo[:, tts(tt)], False, True)
                    hb = hdn[:, ui % 2]
                    for hc in range(4):
                        pg = next_ps()
                        pu = next_ps()
                        for k in range(NC8):
                            S.mm(pg[:], wg[:, b, k, hc * 128:(hc + 1) * 128], xn(k, tt * TT, TT), k == 0, k == NC8 - 1)
                        for k in range(NC8):
                            S.mm(pu[:], wu[:, b, k, hc * 128:(hc + 1) * 128], xn(k, tt * TT, TT), k == 0, k == NC8 - 1)
                        r = rr[0] % 3
                        rr[0] += 1
                        S.act(sgs[:, r, :], pg[:], AF.Silu)
                        S.tt('dve', tus[:, r, :], pu[:], sgs[:, r, :], ALU.mult)
                        S.tt('dve', hb[:, hc, :], tus[:, r, :], pcb[:], ALU.mult)
                        if False:
                            dbt = sb("dbt", [128, 4, TT], F32, pc)
                            S.copy('dve', dbt[:, 0, :], pcb[:])
                            S.copy('act', dbt[:, 1, :], pg[:])
                            S.copy('dve', dbt[:, 2, :], pu[:])
                            S.copy('dve', dbt[:, 3, :], hb[:, hc, :])
                            dump(dbt[:].rearrange("p a b -> p (a b)"), 4096)

                def down(u, ui):
                    e, tt = u
                    b = e % 2
                    hb = hdn[:, ui % 2]
                    for dc in range(NC8):
                        pd = next_ps()
                        for hc in range(4):
                            S.mm(pd[:], wd[:, b, hc, dc * 128:(dc + 1) * 128], hb[:, hc, :], hc == 0, hc == 3)
                        if dbg is not None and ui == 5 and dc == 2:
                            dbt2 = sb("dbt2", [128, 1, TT], F32, pc)
                            S.copy('dve', dbt2[:, 0, :], pd[:])
                            dump(dbt2[:].rearrange("p a b -> p (a b)"), 4096)
                            dbt3 = sb("dbt3", [128, 4, TT], BF16, pc)
                            for hc in range(4):
                                S.copy('dve', dbt3[:, hc, :], hb[:, hc, :])
                            dump(dbt3[:].rearrange("p a b -> p (a b)").bitcast(F32), 4608)
                        S.tt('dve', h[:, dc, tts(tt)], pd[:], h[:, dc, tts(tt)], ALU.add)

                load_w(0)
                ps_set[0] = list(range(6))
                for ui, u in enumerate(units):
                    e, tt = u
                    if tt == 0 and e + 1 < NE:
                        pass
                    gu(u, ui)
                    if DBG_BARRIER:
                        barrier()
                    if ui > 0:
                        down(units[ui - 1], ui - 1)
                    if DBG_BARRIER:
                        barrier()
                    if tt == 0 and e + 1 < NE:
                        load_w(e + 1)
                down(units[-1], len(units) - 1)
                ps_set[0] = list(range(8))
            barrier()

        def phase_C(l):
            with ExitStack() as pc:
                wpg = sb(f"wpg_{l}", [128, NC8, D], BF16, pc)
                wpp = sb(f"wpp_{l}", [128, 2, D], BF16, pc)
                ptb = sb(f"ptb_{l}", [128, 2, S_LEN], BF16, pc)
                sgc = sb(f"sgc_{l}", [128, 3, TT], F32, pc)
                tc_ = sb(f"tc_{l}", [128, 3, TT], F32, pc)
                wv = w_pg[l].rearrange("(c p) n -> p c n", p=128)
                for c in range(NC8):
                    S.dma('pool', wpg[:, c, :], wv[:, c, :])
                S.dma('pool', wpp[:], w_pp[l].rearrange("(c p) n -> p c n", p=128))
                pv = pT[l].rearrange("(c p) t -> p c t", p=128)
                for c in range(2):
                    S.dma('pool', ptb[:, c, :], pv[:, c, :])
                rms_tile(0, 'g_ple', l, lambda c: xn(c, 0, TT))
                r = 0
                for tt in range(NTT):
                    if tt + 1 < NTT:
                        rms_tile(tt + 1, 'g_ple', l, lambda c, t1=tt + 1: xn(c, t1 * TT, TT))
                    for co in range(NC8):
                        pg = next_ps()
                        pp = next_ps()
                        for k in range(NC8):
                            S.mm(pg[:], wpg[:, k, co * 128:(co + 1) * 128], xn(k, tt * TT, TT), k == 0, k == NC8 - 1)
                        for k in range(2):
                            S.mm(pp[:], wpp[:, k, co * 128:(co + 1) * 128], ptb[:, k, tts(tt)], k == 0, k == 1)
                        S.act(sgc[:, r % 3, :], pg[:], AF.Sigmoid)
                        S.tt('dve', tc_[:, r % 3, :], pp[:], sgc[:, r % 3, :], ALU.mult)
                        S.tt('pool', h[:, co, tts(tt)], h[:, co, tts(tt)], tc_[:, r % 3, :], ALU.add)
                        r += 1
            barrier()

        def phase_D():
            SCALE = HD ** -0.5
            with ExitStack() as pc:
                ao = sb("ao", [128, NH, S_LEN], BF16, pc)
                wh = sb("wh", [128, 2, 3, NC8, HD], BF16, pc)
                qT_ = sb("qT", [128, S_LEN], BF16, pc)
                kT_ = sb("kT", [128, S_LEN], BF16, pc)
                vh = sb("vh", [128, 16, HD], BF16, pc)
                q32 = sb("q32", [128, 2, TT], F32, pc)
                ksum = sb("ksum", [128, 8], F32, pc)
                gs = sb("gs", [128, 8, 8], F32, pc)
                top8 = sb("top8", [128, 8, 8], F32, pc)
                ext = sb("ext", [128, 16, 40], F32, pc)
                rh = sb("rh", [128, S_LEN], BF16, pc)
                rtD1 = sb("rtD1", [128, TT], F32, pc)
                rsD1 = sb("rsD1", [128, TT], F32, pc)
                rtD = [rt, rtD1]
                rsD = [rstd, rsD1]
                pt_ = sb("pt", [128, 4, 256], BF16, pc)
                rden = sb("rden", [128, 2, 256], F32, pc)
                wo = sb("wo", [128, NC8, D], BF16, pc)
                rms_tile(0, 'g_mix', 1, lambda c: xn(c, 0, TT))
                rms_tile(1, 'g_mix', 1, lambda c: xn(c, TT, TT))
                rms_pending = [2, 3]
                S.memset('pool', ext[:], 0.0)
                S.memset('pool', rh[:], 0.0)
                wqv = w_qkv.rearrange("(c p) n -> p c n", p=128)

                def load_head(hh):
                    b = hh % 2
                    for j in range(3):
                        S.dma('pool', wh[:, b, j], wqv[:, :, j * D + hh * HD:j * D + (hh + 1) * HD])

                load_head(0)
                wov = w_o.rearrange("(c p) n -> p c n", p=128)
                for c in range(NC8):
                    S.dma('pool', wo[:, c, :], wov[:, c, :])
                gen = [0, 1, 2]
                for hh in range(NH):
                    b = hh % 2
                    if hh + 1 < NH:
                        load_head(hh + 1)
                    ps_set[0] = [0, 1, 2, 4, 5, 6, 7]
                    psg = PS[3]
                    tiles = [(1, tt) for tt in range(NTT)] + [(0, tt) for tt in range(NTT)]
                    pst = {}

                    def stage1(n):
                        j, tt = tiles[n]
                        ps = next_ps()
                        for k in range(NC8):
                            S.mm(ps[:], wh[:, b, j, k, :], xn(k, tt * TT, TT), k == 0, k == NC8 - 1)
                        S.act(sq[:, n % 2, :], ps[:], AF.Square)
                        p2_ = next_ps()
                        S.mm(p2_[:], ones_bf[:], sq[:, n % 2, :], True, True)
                        S.act(rtD[n % 2][:], p2_[:], AF.Ln, bias=EPS, scale=1.0 / HD)
                        S.act(rsD[n % 2][:], rtD[n % 2][:], AF.Exp, scale=-0.5)
                        pst[n] = ps

                    def stage2(n):
                        j, tt = tiles[n]
                        ps = pst[n]
                        d32 = q32[:, n % 2, :]
                        if j == 1:
                            S.stt(d32, ps[:], P('k_gain'), rsD[n % 2][:], ALU.mult, ALU.mult)
                            S.copy('act', kT_[:, tts(tt)], d32)
                            S.reduce(ksum[:, 2 * tt:2 * tt + 2], d32.rearrange("p (a b) -> p a b", b=256), ALU.add)
                        else:
                            S.stt(d32, ps[:], P('q_gain'), rsD[n % 2][:], ALU.mult, ALU.mult)
                            S.act(qT_[:, tts(tt)], d32, AF.Copy, scale=SCALE)
                            if tt >= 2:
                                for i4 in range(4):
                                    i8 = tt * 4 + i4 - 8
                                    S.mm(psg[:, i8 * 8:(i8 + 1) * 8], d32[:, i4 * 128:(i4 + 1) * 128], ksum[:], True, True)

                    stage1(0)
                    for n in range(len(tiles)):
                        if n + 1 < len(tiles):
                            stage1(n + 1)
                        stage2(n)
                        while n == 0 and rms_pending:
                            t1 = rms_pending.pop(0)
                            rms_tile(t1, 'g_mix', 1, lambda c, t1=t1: xn(c, t1 * TT, TT))
                    ps_set[0] = gen
                    S.tt('dve', gs[:], psg[:, 0:64].rearrange("p (a b) -> p a b", b=8),
                         cst[:, CO['negm']:CO['negm'] + 64].rearrange("p (a b) -> p a b", b=8), ALU.add)
                    for i8 in range(8):
                        S.op('dve', lambda e, i8=i8: e.max(out=top8[:, i8, :], in_=gs[:, i8, :]),
                             ins=[gs[:, i8, :]], outs=[top8[:, i8, :]])
                    for i8 in range(8):
                        S.ts('dve', ext[:, 8 + i8, 32:40], gs[:, i8, :], top8[:, i8, 2:3], ALU.is_lt, -30000.0, ALU.mult)
                    S.copy('dve', ext[:, :, 0].rearrange("p (a b) -> p a b", b=2),
                           cst[:, CO['alc'] + hh * 2:CO['alc'] + hh * 2 + 2].unsqueeze(1).to_broadcast([128, 8, 2]))
                    for g4 in range(4):
                        ps = next_ps()
                        for i4 in range(4):
                            i = g4 * 4 + i4
                            S.transpose(ps[0:40, i4 * 128:(i4 + 1) * 128], ext[:, i, :], ident)
                        S.copy('act', rh[0:40, g4 * 512:(g4 + 1) * 512], ps[0:40, :])
                    for g4 in range(4):
                        ps = next_ps()
                        for i4 in range(4):
                            i = g4 * 4 + i4
                            for k in range(NC8):
                                S.mm(ps[:, i4 * 128:(i4 + 1) * 128], xn(k, i * 128, 128), wh[:, b, 2, k, :],
                                     k == 0, k == NC8 - 1)
                        S.copy('act', vh[:, g4 * 4:(g4 + 1) * 4, :].rearrange("p a b -> p (a b)"), ps[:])
                    prr = [0]
                    for jq in range(8):
                        po = PS[4 + jq % 2]
                        pd = PS[6 + jq % 2]
                        kts = list(range(2 * jq + 2))
                        info = []
                        for kt in kts:
                            n = kt // 2
                            own = (n == jq)
                            second = own and (kt == 2 * jq + 1)
                            c0 = 128 if second else 0
                            nq = 128 if second else 256
                            ssel = 8 if (own or jq <= 3) else n
                            info.append((kt, own, c0, nq, ssel))
                        stile = {}

                        def emit_s(idx):
                            kt, own, c0, nq, ssel = info[idx]
                            ps = next_ps()
                            q0 = jq * 256 + c0
                            S.mm(ps[:, 0:nq], kT_[:, kt * 128:(kt + 1) * 128], qT_[:, q0:q0 + nq], True, False)
                            S.mm(ps[:, 0:nq], esel_bf[:, ssel, :], rh[:, q0:q0 + nq], False, True)
                            pb = pt_[:, prr[0] % 4, :]
                            prr[0] += 1
                            dlt = 2 * jq - kt + 1
                            S.act(pb[:, 0:nq], ps[:, 0:nq], AF.Exp,
                                  bias=cst[:, CO['ab'] + hh * 17 + dlt:CO['ab'] + hh * 17 + dlt + 1])
                            if own:
                                S.tt('pool', pb[:, 0:128], pb[:, 0:128], tri_bf[:], ALU.mult)
                            stile[idx] = pb

                        def emit_pv(idx):
                            kt, own, c0, nq, ssel = info[idx]
                            pb = stile[idx]
                            first = idx == 0
                            last = idx == len(info) - 1
                            S.mm(po[:, c0:c0 + nq], vh[:, kt, :], pb[:, 0:nq], first, last, inc=last)
                            S.mm(pd[:, c0:c0 + nq], ones_bf[:], pb[:, 0:nq], first, last, inc=True)

                        LA = 2
                        for idx in range(min(LA, len(info))):
                            emit_s(idx)
                        for idx in range(len(info)):
                            if idx + LA < len(info):
                                emit_s(idx + LA)
                            emit_pv(idx)
                        S.act(rden[:, jq % 2, :], pd[:, 0:256], AF.Ln)
                        S.act(rden[:, jq % 2, :], rden[:, jq % 2, :], AF.Exp, scale=-1.0)
                        S.tt('dve', ao[:, hh, jq * 256:(jq + 1) * 256], po[:, 0:256], rden[:, jq % 2, :], ALU.mult)
                ps_set[0] = list(range(8))
                for tt in range(NTT):
                    for co in range(NC8):
                        ps = next_ps()
                        for k in range(NC8):
                            S.mm(ps[:], wo[:, k, co * 128:(co + 1) * 128], ao[:, k, tts(tt)], k == 0, k == NC8 - 1)
                        S.tt('dve', h[:, co, tts(tt)], ps[:], h[:, co, tts(tt)], ALU.add)
            barrier()

        barrier()
        for ph in phases:
            if ph == 'A':
                phase_A()
            elif ph == 'B0':
                phase_B(0)
            elif ph == 'B1':
                phase_B(1)
            elif ph == 'C0':
                phase_C(0)
            elif ph == 'C1':
                phase_C(1)
            elif ph == 'D':
                phase_D()

        yv = yT.rearrange("(c p) t -> p c t", p=128)
        toks = []
        for c in range(NC8):
            toks.append(S.dma('sp' if c % 2 == 0 else 'act', yv[:, c, :], h[:, c, :]))
        for t in toks + dbg_toks:
            S.wait_tok('sp', t)
        build_program.stats = (S.nops, S.nwaits)
    return nc


_CACHE = {}


LAST = {}


def run(inputs, phases=('A', 'B0', 'C0', 'D', 'B1', 'C1'), debug=False):
    key = (tuple(phases), debug)
    if key not in _CACHE:
        _CACHE[key] = build_program(phases, debug)
    nc = _CACHE[key]
    x = np.asarray(inputs['x'], dtype=np.float32)
    p = np.asarray(inputs['p'], dtype=np.float32)
    prm = pack_params(inputs)
    cst = make_consts()
    shared = {
        "prm": prm, "cst": cst,
        "w_pw1": np.ascontiguousarray(np.asarray(inputs['conv_w_pw1'], dtype=np.float32)[0]),
        "w_pw2": np.ascontiguousarray(np.asarray(inputs['conv_w_pw2'], dtype=np.float32)[0]),
        "w_qkv": np.ascontiguousarray(np.asarray(inputs['attn_w_qkv'], dtype=np.float32)[0]),
        "w_o": np.ascontiguousarray(np.asarray(inputs['attn_w_o'], dtype=np.float32)[0]),
        "w_gate": np.ascontiguousarray(np.asarray(inputs['moe_w_gate'], dtype=np.float32)),
        "w_up": np.ascontiguousarray(np.asarray(inputs['moe_w_up'], dtype=np.float32)),
        "w_down": np.ascontiguousarray(np.asarray(inputs['moe_w_down'], dtype=np.float32)),
        "w_pg": np.ascontiguousarray(np.asarray(inputs['ple_w_gate'], dtype=np.float32)),
        "w_pp": np.ascontiguousarray(np.asarray(inputs['ple_w_proj'], dtype=np.float32)),
    }
    in_maps = []
    for b in range(NCORES):
        m = dict(shared)
        m["xT"] = np.ascontiguousarray(x[b].T)
        m["pT"] = np.ascontiguousarray(p[:, b].transpose(0, 2, 1))
        in_maps.append(m)
    res = run_bass_kernel_spmd(nc, in_maps, core_ids=list(range(NCORES)))
    if debug:
        LAST['dbg'] = [np.asarray(res.results[b]["dbg"]) for b in range(NCORES)]
    out = np.stack([np.asarray(res.results[b]["yT"], dtype=np.float32).T for b in range(NCORES)], axis=0)
    return np.ascontiguousarray(out)


def kernel(**inputs):
    return run(inputs)
```

```python
import math
from contextlib import ExitStack

import numpy as np
import concourse.bass as bass
import concourse.mybir as mybir
from concourse.bass_utils import run_bass_kernel_spmd

F32 = mybir.dt.float32
BF16 = mybir.dt.bfloat16
AF = mybir.ActivationFunctionType
ALU = mybir.AluOpType
AX = mybir.AxisListType

D = 1024
S_LEN = 2048
NC8 = 8
NTT = 4
TT = 512
HALO = 30
NH = 8
HD = 128
NE = 16
EH = 512
PLE = 256
EPS = 1e-6
NCORES = 8
DBG_BARRIER = False


class Sched:
    def __init__(self, nc, ctx):
        self.nc = nc
        self.ctx = ctx
        self.eng = {'pe': nc.tensor, 'act': nc.scalar, 'dve': nc.vector, 'pool': nc.gpsimd, 'sp': nc.sync}
        self.sem = {}
        self.cnt = {}
        self.nsem = 0
        for e in ['pe', 'act', 'dve', 'pool']:
            self._new_sem(e)
        self.seen = {e: {} for e in self.eng}
        self.W = {}
        self.R = {}
        self.pend = {e: [] for e in self.eng}
        self.dma_sems = []
        self.dma_cnt = []
        self.dma_rr = {'hw': 0, 'sw': 0}
        self.dma_pool = {'hw': list(range(0, 8)), 'sw': list(range(8, 32))}
        for i in range(32):
            self.dma_sems.append(ctx.enter_context(nc.semaphore(f"dq{i}")))
            self.dma_cnt.append(0)
        self.nwaits = 0
        self.nops = 0

    def _new_sem(self, e):
        self.nsem += 1
        self.sem[e] = self.ctx.enter_context(self.nc.semaphore(f"s_{e}_{self.nsem}"))
        self.cnt[e] = 0

    @staticmethod
    def regions(ap):
        t = ap.tensor
        if str(ap.space) == 'DRAM':
            return []
        shape = t.shape
        row = 1
        for s in shape[1:]:
            row *= s
        off = int(ap.offset)
        dims = ap.ap
        p0 = off // row
        f0 = off % row
        npart = dims[0][1]
        fd = sorted([(abs(s), n) for (s, n) in dims[1:] if n > 1 and s != 0], reverse=True)

        def expand(ds, base, budget):
            if not ds:
                return [(base, base + 1)]
            s0, n0 = ds[0]
            inner = 1
            for (t_, m_) in ds[1:]:
                inner += (m_ - 1) * t_
            if s0 > inner and n0 <= budget:
                out = []
                for i in range(n0):
                    out += expand(ds[1:], base + i * s0, budget // n0)
                return out
            return [(base, base + inner + (n0 - 1) * s0)]

        name = t.name
        return [(name, p0, p0 + npart, a, b) for (a, b) in expand(fd, f0, 32)]

    @staticmethod
    def _ov(a, r):
        return a[0] < r[2] and r[1] < a[1] and a[2] < r[4] and r[3] < a[3]

    def _collect(self, engine, ins, outs):
        toks = []
        rin = [r for a in ins for r in self.regions(a)]
        rout = [r for a in outs for r in self.regions(a)]
        same = engine == 'pe'
        for r in rin:
            if r is None:
                continue
            for w in self.W.get(r[0], ()):
                if self._ov(w, r):
                    toks.append(w[4])
        for r in rout:
            if r is None:
                continue
            for w in self.W.get(r[0], ()):
                if self._ov(w, r) and not (same and w[4][2] == engine):
                    toks.append(w[4])
            for rd in self.R.get(r[0], ()):
                if self._ov(rd, r) and not (same and rd[4][2] == engine):
                    toks.append(rd[4])
        return toks, rin, rout

    def _wait(self, engine, toks):
        need = {}
        for (sem, val, src) in toks:
            k = id(sem)
            if k not in need or need[k][1] < val:
                need[k] = (sem, val, src)
        for k, (sem, val, src) in need.items():
            if src == engine and engine == 'pe':
                continue
            if self.seen[engine].get(k, 0) < val:
                self.eng[engine].wait_ge(sem, val)
                self.seen[engine][k] = val
                self.nwaits += 1

    def _record(self, tok, rin, rout):
        for r in rin:
            if r is None:
                continue
            lst = self.R.setdefault(r[0], [])
            lst[:] = [x for x in lst if not (x[4][0] is tok[0] and x[0] >= r[1] and x[1] <= r[2]
                                             and x[2] >= r[3] and x[3] <= r[4])]
            lst.append((r[1], r[2], r[3], r[4], tok))
        for r in rout:
            if r is None:
                continue
            lst = self.W.setdefault(r[0], [])
            lst[:] = [x for x in lst if not (x[0] >= r[1] and x[1] <= r[2] and x[2] >= r[3] and x[3] <= r[4])]
            lst.append((r[1], r[2], r[3], r[4], tok))
            rl = self.R.get(r[0])
            if rl:
                rl[:] = [x for x in rl if not (x[0] >= r[1] and x[1] <= r[2] and x[2] >= r[3] and x[3] <= r[4])]

    def op(self, engine, fn, ins=(), outs=(), inc=True):
        toks, rin, rout = self._collect(engine, ins, outs)
        self._wait(engine, toks)
        ins_obj = fn(self.eng[engine])
        self.nops += 1
        if inc:
            if self.cnt[engine] >= 30000:
                self._new_sem(engine)
            self.cnt[engine] += 1
            ins_obj.then_inc(self.sem[engine], 1)
            tok = (self.sem[engine], self.cnt[engine], engine)
            for (pi, po) in self.pend[engine]:
                self._record(tok, pi, po)
            self.pend[engine] = []
            self._record(tok, rin, rout)
            return tok
        else:
            self.pend[engine].append((rin, rout))
            return None

    def dma(self, queue, out, in_, **kw):
        kind = 'sw' if queue == 'pool' else 'hw'
        lst = self.dma_pool[kind]
        i = lst[self.dma_rr[kind] % len(lst)]
        self.dma_rr[kind] += 1
        sem = self.dma_sems[i]
        toks, rin, rout = self._collect('dma', [in_], [out])
        if self.dma_cnt[i] > 0:
            toks.append((sem, self.dma_cnt[i], 'dma'))
        self._wait(queue, toks)
        ins_obj = self.eng[queue].dma_start(out=out, in_=in_, **kw)
        self.dma_cnt[i] += 16
        ins_obj.then_inc(sem, 16)
        tok = (sem, self.dma_cnt[i], 'dma')
        self._record(tok, rin, rout)
        return tok

    def wait_tok(self, engine, tok):
        self._wait(engine, [tok])

    def mm(self, out, lhsT, rhs, start, stop, inc=None):
        inc = True
        return self.op('pe', lambda e: e.matmul(out, lhsT=lhsT, rhs=rhs, start=start, stop=stop),
                       ins=[lhsT, rhs], outs=[out], inc=inc)

    def transpose(self, out, in_, ident):
        return self.op('pe', lambda e: e.transpose(out, in_, ident), ins=[in_, ident], outs=[out])

    def act(self, out, in_, func, bias=None, scale=None, eng='act'):
        ins = [in_]
        kw = {}
        if bias is not None:
            kw['bias'] = bias
            if not isinstance(bias, (int, float)):
                ins.append(bias)
        if scale is not None:
            kw['scale'] = scale
            if not isinstance(scale, (int, float)):
                ins.append(scale)
        return self.op('act', lambda e: e.activation(out=out, in_=in_, func=func, **kw), ins=ins, outs=[out])

    def tt(self, eng, out, in0, in1, op):
        return self.op(eng, lambda e: e.tensor_tensor(out=out, in0=in0, in1=in1, op=op), ins=[in0, in1], outs=[out])

    def ts(self, eng, out, in0, s1, op0, s2=None, op1=None):
        ins = [in0]
        if not isinstance(s1, (int, float)):
            ins.append(s1)
        if s2 is not None and not isinstance(s2, (int, float)):
            ins.append(s2)
        if op1 is None:
            return self.op(eng, lambda e: e.tensor_scalar(out=out, in0=in0, scalar1=s1, scalar2=None, op0=op0),
                           ins=ins, outs=[out])
        return self.op(eng, lambda e: e.tensor_scalar(out=out, in0=in0, scalar1=s1, scalar2=s2, op0=op0, op1=op1),
                       ins=ins, outs=[out])

    def stt(self, out, in0, scalar, in1, op0, op1):
        ins = [in0, in1]
        if not isinstance(scalar, (int, float)):
            ins.append(scalar)
        return self.op('dve', lambda e: e.scalar_tensor_tensor(out=out, in0=in0, scalar=scalar, in1=in1, op0=op0, op1=op1),
                       ins=ins, outs=[out])

    def copy(self, eng, out, in_):
        if eng == 'act':
            return self.op('act', lambda e: e.copy(out=out, in_=in_), ins=[in_], outs=[out])
        return self.op(eng, lambda e: e.tensor_copy(out=out, in_=in_), ins=[in_], outs=[out])

    def recip(self, out, in_):
        return self.op('dve', lambda e: e.reciprocal(out=out, in_=in_), ins=[in_], outs=[out])

    def recip_fast(self, out, in_):
        return self.op('dve', lambda e: e.reciprocal_approx_fast(out=out, in_=in_), ins=[in_], outs=[out])

    def reduce(self, out, in_, op):
        return self.op('dve', lambda e: e.tensor_reduce(out=out, in_=in_, axis=AX.X, op=op), ins=[in_], outs=[out])

    def memset(self, eng, ap, val):
        return self.op(eng, lambda e: e.memset(ap, val), ins=[], outs=[ap])


class Packer:
    def __init__(self):
        self.cols = []
        self.off = {}
        self.n = 0

    def add(self, name, arr):
        arr = np.ascontiguousarray(arr, dtype=np.float32)
        assert arr.shape[0] == 128, (name, arr.shape)
        arr = arr.reshape(128, -1)
        self.off[name] = self.n
        self.n += arr.shape[1]
        self.cols.append(arr)

    def build(self):
        return np.ascontiguousarray(np.concatenate(self.cols, axis=1))


def feat_cols(v):
    v = np.asarray(v, dtype=np.float32)
    return v.reshape(-1, 128).T


def prm_layout():
    off = {}
    n = 0
    for name, w in [('g_mix', 16), ('g_ffn', 16), ('g_ple', 16), ('b_pw1', 16), ('w_dw', 248), ('b_dw', 8),
                    ('ln_g', 8), ('ln_b', 8), ('b_pw2', 8), ('q_gain', 1), ('k_gain', 1),
                    ('wr0', 160), ('wr1', 160), ('rb0', 20), ('rb1', 20)]:
        off[name] = n
        n += w
    return off, n


def pack_params(inp):
    pk = Packer()
    pk.add('g_mix', feat_cols(inp['g_mix']))
    pk.add('g_ffn', feat_cols(inp['g_ffn']))
    pk.add('g_ple', feat_cols(inp['g_ple']))
    pk.add('b_pw1', feat_cols(inp['conv_b_pw1'][0]))
    wdw = np.asarray(inp['conv_w_dw'][0], dtype=np.float32)
    pk.add('w_dw', wdw.reshape(31, 8, 128).transpose(2, 1, 0).reshape(128, 248))
    pk.add('b_dw', feat_cols(inp['conv_b_dw'][0]))
    pk.add('ln_g', feat_cols(inp['conv_ln_g'][0]))
    pk.add('ln_b', feat_cols(inp['conv_ln_b'][0]))
    pk.add('b_pw2', feat_cols(inp['conv_b_pw2'][0]))
    pk.add('q_gain', np.asarray(inp['attn_q_gain'][0], dtype=np.float32).reshape(128, 1))
    pk.add('k_gain', np.asarray(inp['attn_k_gain'][0], dtype=np.float32).reshape(128, 1))
    for l in range(2):
        wg = np.asarray(inp['moe_w_group'][l], dtype=np.float32)
        wr = np.asarray(inp['moe_w_router'][l], dtype=np.float32)
        wall = np.concatenate([wg, wr.transpose(1, 0, 2).reshape(1024, 16)], axis=1)
        pk.add(f'wr{l}', wall.reshape(8, 128, 20).transpose(1, 0, 2).reshape(128, 160))
    for l in range(2):
        b = np.concatenate([np.asarray(inp['moe_b_group'][l], dtype=np.float32).reshape(4),
                            np.asarray(inp['moe_b_router'][l], dtype=np.float32).reshape(16)])
        pk.add(f'rb{l}', np.tile(b[None, :], (128, 1)))
    arr = pk.build()
    off, n = prm_layout()
    assert off == pk.off and n == arr.shape[1]
    return arr


def cst_layout():
    off = {}
    n = 0
    for name, w in [('ident', 128), ('tri', 128), ('ab', 8 * 17), ('alc', 16), ('negm', 64), ('esel', 9 * 128)]:
        off[name] = n
        n += w
    return off, n


def make_consts():
    pk = Packer()
    pk.add('ident', np.eye(128, dtype=np.float32))
    p = np.arange(128)[:, None]
    f = np.arange(128)[None, :]
    pk.add('tri', (f >= p).astype(np.float32))
    slopes = np.array([2.0 ** (-(i + 1)) for i in range(8)], dtype=np.float64)
    ab = np.zeros((128, 8, 17), dtype=np.float64)
    for h in range(8):
        for d in range(17):
            ab[:, h, d] = slopes[h] * (np.arange(128) - 128.0 * (d - 1))
    pk.add('ab', ab.reshape(128, 136))
    alc = np.zeros((128, 8, 2), dtype=np.float64)
    for h in range(8):
        for par in range(2):
            alc[:, h, par] = -slopes[h] * (par * 128 + np.arange(128))
    pk.add('alc', alc.reshape(128, 16))
    negm = np.zeros((128, 8, 8), dtype=np.float32)
    for i8 in range(8):
        j = (8 + i8) // 2
        for n in range(8):
            if n >= j:
                negm[:, i8, n] = -1e30
    pk.add('negm', negm.reshape(128, 64))
    esel = np.zeros((128, 9, 128), dtype=np.float32)
    for s in range(9):
        esel[0, s, :] = 1.0
        if s < 8:
            esel[32 + s, s, :] = 1.0
    pk.add('esel', esel.reshape(128, 9 * 128))
    arr = pk.build()
    off, n = cst_layout()
    assert off == pk.off and n == arr.shape[1]
    return arr


def build_program(phases=('A', 'B0', 'C0', 'D', 'B1', 'C1'), debug=False):
    nc = bass.Bass("TRN2", target_bir_lowering=False)
    PO, NPRM = prm_layout()
    CO, NCST = cst_layout()

    def din(name, shape):
        return nc.dram_tensor(name, list(shape), F32, kind="ExternalInput").ap()

    xT = din("xT", [D, S_LEN])
    pT = din("pT", [2, PLE, S_LEN])
    prm_d = din("prm", [128, NPRM])
    cst_d = din("cst", [128, NCST])
    w_pw1 = din("w_pw1", [D, 2 * D])
    w_pw2 = din("w_pw2", [D, D])
    w_qkv = din("w_qkv", [D, 3 * D])
    w_o = din("w_o", [D, D])
    w_gate = din("w_gate", [2, NE, D, EH])
    w_up = din("w_up", [2, NE, D, EH])
    w_down = din("w_down", [2, NE, EH, D])
    w_pg = din("w_pg", [2, D, D])
    w_pp = din("w_pp", [2, PLE, D])
    yT = nc.dram_tensor("yT", [D, S_LEN], F32, kind="ExternalOutput").ap()
    dbg = nc.dram_tensor("dbg", [128, 8192], F32, kind="ExternalOutput").ap() if debug else None

    with ExitStack() as ctx:
        S = Sched(nc, ctx)

        def sb(name, shape, dt, c=ctx):
            return c.enter_context(nc.sbuf_tensor(name, list(shape), dt))

        h = sb("h", [128, NC8, S_LEN], F32)
        xg = sb("xg", [128, NC8, HALO + S_LEN], BF16)
        prm = sb("prm_s", [128, NPRM], F32)
        cst = sb("cst_s", [128, NCST], F32)
        ones_bf = sb("ones_bf", [128, 128], BF16)
        tri_bf = sb("tri_bf", [128, 128], BF16)
        esel_bf = sb("esel_bf", [128, 9, 128], BF16)
        sq = sb("sq", [128, 2, TT], BF16)
        rt = sb("rt", [128, TT], F32)
        rstd = sb("rstd", [128, TT], F32)
        PS = [ctx.enter_context(nc.psum_tensor(f"ps{i}", [128, 512], F32)) for i in range(8)]
        ps_rr = [0]
        ps_set = [list(range(8))]

        def next_ps():
            lst = ps_set[0]
            b = lst[ps_rr[0] % len(lst)]
            ps_rr[0] += 1
            return PS[b]

        ident = cst[:, CO['ident']:CO['ident'] + 128]

        def xn(c, lo, n):
            return xg[:, c, HALO + lo:HALO + lo + n]

        def P(name, j=0, n=1):
            return prm[:, PO[name] + j:PO[name] + j + n]

        S.dma('sp', cst[:], cst_d)
        S.dma('sp', prm[:], prm_d)
        xv = xT.rearrange("(c p) t -> p c t", p=128)
        for tt0 in range(NTT):
            for c in range(NC8):
                S.dma('sp' if c % 2 == 0 else 'act', h[:, c, tt0 * TT:(tt0 + 1) * TT], xv[:, c, tt0 * TT:(tt0 + 1) * TT])
        S.memset('dve', ones_bf[:], 1.0)
        S.copy('dve', tri_bf[:], cst[:, CO['tri']:CO['tri'] + 128])
        S.copy('dve', esel_bf[:].rearrange("p a b -> p (a b)"), cst[:, CO['esel']:CO['esel'] + 9 * 128])
        S.memset('pool', xg[:, :, 0:HALO], 0.0)

        def tts(tt):
            return slice(tt * TT, (tt + 1) * TT)

        dbg_toks = []

        def dump(ap, col):
            if dbg is None:
                return
            np_, n = ap.shape[0], ap.shape[1]
            dbg_toks.append(S.dma('sp', dbg[0:np_, col:col + n], ap))

        def rms_tile(tt, gname, gl, out_fn):
            ps = next_ps()
            for c in range(NC8):
                S.act(sq[:, c % 2, :], h[:, c, tts(tt)], AF.Square)
                S.mm(ps[:], ones_bf[:], sq[:, c % 2, :], start=(c == 0), stop=(c == NC8 - 1))
            S.act(rt[:], ps[:], AF.Ln, bias=EPS, scale=1.0 / D)
            S.act(rstd[:], rt[:], AF.Exp, scale=-0.5)
            for c in range(NC8):
                S.stt(out_fn(c), h[:, c, tts(tt)], P(gname, gl * 8 + c), rstd[:], ALU.mult, ALU.mult)

        def barrier():
            toks = []
            for e in ['pe', 'act', 'dve', 'pool']:
                if S.cnt[e] > 0:
                    toks.append((S.sem[e], S.cnt[e], e))
            for i, sem in enumerate(S.dma_sems):
                if S.dma_cnt[i] > 0:
                    toks.append((sem, S.dma_cnt[i], 'dma'))
            for e in ['pe', 'act', 'dve', 'pool', 'sp']:
                S._wait(e, toks)

        def phase_A():
            with ExitStack() as pc:
                rms_tile(0, 'g_mix', 0, lambda c: xn(c, 0, TT))
                glu = sb("glu", [128, NC8, HALO + S_LEN], BF16, pc)
                S.memset('pool', glu[:, :, 0:HALO], 0.0)
                with ExitStack() as p2:
                    w1 = sb("w1", [128, NC8, 2 * D], BF16, p2)
                    sg = sb("sgA", [128, 2, TT], F32, p2)
                    w1v = w_pw1.rearrange("(c p) n -> p c n", p=128)
                    for c in range(NC8):
                        S.dma('pool', w1[:, c, :], w1v[:, c, :])
                    for tt in range(NTT):
                        if tt + 1 < NTT:
                            rms_tile(tt + 1, 'g_mix', 0, lambda c, t1=tt + 1: xn(c, t1 * TT, TT))
                        for c in range(NC8):
                            pa = next_ps()
                            pg = next_ps()
                            for k in range(NC8):
                                S.mm(pa[:], w1[:, k, c * 128:(c + 1) * 128], xn(k, tt * TT, TT), k == 0, k == NC8 - 1)
                            for k in range(NC8):
                                S.mm(pg[:], w1[:, k, D + c * 128:D + (c + 1) * 128], xn(k, tt * TT, TT), k == 0, k == NC8 - 1)
                            S.act(sg[:, c % 2, :], pg[:], AF.Sigmoid, bias=P('b_pw1', 8 + c))
                            S.stt(glu[:, c, HALO + tt * TT:HALO + (tt + 1) * TT], pa[:], P('b_pw1', c), sg[:, c % 2, :],
                                  ALU.add, ALU.mult)
                    barrier()
                dg = sb("dg", [128, 2, 31, 128], BF16, pc)
                w2 = sb("w2", [128, NC8, D], BF16, pc)
                w2v = w_pw2.rearrange("(c p) n -> p c n", p=128)
                for c in range(NC8):
                    S.dma('pool', w2[:, c, :], w2v[:, c, :])

                def cvb(c, tt):
                    return xn(c, tt * TT, TT)

                for c in range(NC8):
                    wd = prm[:, PO['w_dw'] + c * 31:PO['w_dw'] + (c + 1) * 31]
                    S.tt('dve', dg[:, c % 2], ident.unsqueeze(1).to_broadcast([128, 31, 128]),
                         wd.unsqueeze(2).to_broadcast([128, 31, 128]), ALU.mult)
                    for tt in range(NTT):
                        ps = next_ps()
                        for j in range(31):
                            S.mm(ps[:], dg[:, c % 2, j, :], glu[:, c, tt * TT + j:tt * TT + j + TT], j == 0, j == 30)
                        S.act(cvb(c, tt), ps[:], AF.Identity, bias=P('b_dw', c))
                mean = sb("meanA", [128, TT], F32, pc)
                m2 = sb("m2A", [128, TT], F32, pc)
                var = sb("varA", [128, TT], F32, pc)
                t1 = sb("t1A", [128, 2, TT], F32, pc)
                t2 = sb("t2A", [128, 2, TT], F32, pc)
                for tt in range(NTT):
                    p1 = next_ps()
                    p2_ = next_ps()
                    for c in range(NC8):
                        S.act(sq[:, c % 2, :], cvb(c, tt), AF.Square)
                        S.mm(p2_[:], ones_bf[:], sq[:, c % 2, :], c == 0, c == NC8 - 1)
                    for c in range(NC8):
                        S.mm(p1[:], ones_bf[:], cvb(c, tt), c == 0, c == NC8 - 1)
                    S.ts('dve', mean[:], p1[:], 1.0 / D, ALU.mult)
                    S.tt('dve', m2[:], mean[:], mean[:], ALU.mult)
                    S.stt(var[:], p2_[:], 1.0 / D, m2[:], ALU.mult, ALU.subtract)
                    S.act(rt[:], var[:], AF.Ln, bias=EPS)
                    S.act(rstd[:], rt[:], AF.Exp, scale=-0.5)
                    for c in range(NC8):
                        S.tt('pool', t1[:, c % 2, :], cvb(c, tt), mean[:], ALU.subtract)
                        S.tt('dve', t2[:, c % 2, :], t1[:, c % 2, :], rstd[:], ALU.mult)
                        S.act(xn(c, tt * TT, TT), t2[:, c % 2, :], AF.Silu, bias=P('ln_b', c), scale=P('ln_g', c))
                for tt in range(NTT):
                    for co in range(NC8):
                        ps = next_ps()
                        for k in range(NC8):
                            S.mm(ps[:], w2[:, k, co * 128:(co + 1) * 128], xn(k, tt * TT, TT), k == 0, k == NC8 - 1)
                        S.stt(h[:, co, tts(tt)], ps[:], P('b_pw2', co), h[:, co, tts(tt)], ALU.add, ALU.add)
            barrier()

        def phase_B(l):
            with ExitStack() as pc:
                cT_hi = sb(f"cThi_{l}", [128, S_LEN], BF16, pc)
                cT_lo = sb(f"cTlo_{l}", [128, S_LEN], BF16, pc)
                sel16 = sb(f"sel16_{l}", [128, 16, 128], BF16, pc)
                S.copy('dve', sel16[:], cst[:, CO['ident']:CO['ident'] + 16].unsqueeze(2).to_broadcast([128, 16, 128]))
                wr = prm[:, PO[f'wr{l}']:PO[f'wr{l}'] + 160]
                psL = PS[7]
                ps_set[0] = list(range(7))
                rc = ExitStack()
                combT = sb(f"combT_{l}", [128, S_LEN], F32, rc)
                S.memset('pool', combT[:], 0.0)
                x32 = sb(f"x32_{l}", [128, NC8, TT], F32, rc)
                lb = sb(f"lb_{l}", [128, 16, 20], F32, rc)
                lT = sb(f"lT_{l}", [128, 2, TT], F32, rc)
                for tt in range(NTT):
                    rms_tile(tt, 'g_ffn', l, lambda c: x32[:, c, :])
                    for c in range(NC8):
                        S.copy('act', xn(c, tt * TT, TT), x32[:, c, :])
                    pT_ = next_ps()
                    for c in range(NC8):
                        S.mm(pT_[0:20, :], wr[:, c * 20:(c + 1) * 20], x32[:, c, :], c == 0, c == NC8 - 1)
                    S.copy('act', lT[0:20, tt % 2, :], pT_[0:20, :])
                    for i4 in range(4):
                        i = tt * 4 + i4
                        S.transpose(psL[:, i * 20:(i + 1) * 20], lT[0:20, tt % 2, i4 * 128:(i4 + 1) * 128],
                                    cst[0:20, CO['ident']:CO['ident'] + 20])
                with rc:
                    def T(name, shape):
                        return sb(f"{name}_{l}", shape, F32, rc)
                    rb = prm[:, PO[f'rb{l}']:PO[f'rb{l}'] + 20]
                    S.tt('dve', lb[:], psL[:, 0:320].rearrange("p (t n) -> p t n", n=20),
                         rb.unsqueeze(1).to_broadcast([128, 16, 20]), ALU.add)
                    lg = lb[:, :, 0:4]
                    le = lb[:, :, 4:20].rearrange("p t (g e) -> p t g e", e=4)
                    m = T("r_m", [128, 16])
                    S.reduce(m[:], lg, ALU.max)
                    dd = T("r_d", [128, 16, 4])
                    S.tt('dve', dd[:], lg, m[:].unsqueeze(2).to_broadcast([128, 16, 4]), ALU.subtract)
                    oh = T("r_oh", [128, 16, 4])
                    S.ts('dve', oh[:], dd[:], 0.0, ALU.is_equal)
                    eg = T("r_eg", [128, 16, 4])
                    S.act(eg[:], dd[:], AF.Exp)
                    se = T("r_se", [128, 16])
                    S.reduce(se[:], eg[:], ALU.add)
                    gw = T("r_gw", [128, 16])
                    S.recip(gw[:], se[:])
                    tmp = T("r_tmp", [128, 16, 4, 4])
                    S.tt('dve', tmp[:], le, oh[:].unsqueeze(3).to_broadcast([128, 16, 4, 4]), ALU.mult)
                    sel = T("r_sel", [128, 16, 4])
                    S.reduce(sel[:], tmp[:].rearrange("p t g e -> p t e g"), ALU.add)
                    m1 = T("r_m1", [128, 16])
                    S.reduce(m1[:], sel[:], ALU.max)
                    eq1 = T("r_eq1", [128, 16, 4])
                    S.tt('dve', eq1[:], sel[:], m1[:].unsqueeze(2).to_broadcast([128, 16, 4]), ALU.is_equal)
                    sel2 = T("r_sel2", [128, 16, 4])
                    S.stt(sel2[:].rearrange("p t e -> p (t e)"), eq1[:].rearrange("p t e -> p (t e)"), -1e30,
                          sel[:].rearrange("p t e -> p (t e)"), ALU.mult, ALU.add)
                    m2_ = T("r_m2", [128, 16])
                    S.reduce(m2_[:], sel2[:], ALU.max)
                    eq2 = T("r_eq2", [128, 16, 4])
                    S.tt('dve', eq2[:], sel2[:], m2_[:].unsqueeze(2).to_broadcast([128, 16, 4]), ALU.is_equal)
                    d21 = T("r_d21", [128, 16])
                    S.tt('dve', d21[:], m2_[:], m1[:], ALU.subtract)
                    e21 = T("r_e21", [128, 16])
                    S.act(e21[:], d21[:], AF.Exp)
                    den = T("r_den", [128, 16])
                    S.ts('dve', den[:], e21[:], 1.0, ALU.add)
                    w1_ = T("r_w1", [128, 16])
                    S.recip(w1_[:], den[:])
                    w2_ = T("r_w2", [128, 16])
                    S.tt('dve', w2_[:], e21[:], w1_[:], ALU.mult)
                    w1g = T("r_w1g", [128, 16])
                    S.tt('dve', w1g[:], w1_[:], gw[:], ALU.mult)
                    w2g = T("r_w2g", [128, 16])
                    S.tt('dve', w2g[:], w2_[:], gw[:], ALU.mult)
                    ea = T("r_ea", [128, 16, 4])
                    S.tt('dve', ea[:], eq1[:], w1g[:].unsqueeze(2).to_broadcast([128, 16, 4]), ALU.mult)
                    eb = T("r_eb", [128, 16, 4])
                    S.tt('dve', eb[:], eq2[:], w2g[:].unsqueeze(2).to_broadcast([128, 16, 4]), ALU.mult)
                    ew = T("r_ew", [128, 16, 4])
                    S.tt('dve', ew[:], ea[:], eb[:], ALU.add)
                    comb = T("r_comb", [128, 16, 4, 4])
                    S.tt('dve', comb[:], oh[:].unsqueeze(3).to_broadcast([128, 16, 4, 4]),
                         ew[:].unsqueeze(2).to_broadcast([128, 16, 4, 4]), ALU.mult)
                    dump(lb[:].rearrange("p t n -> p (t n)"), 0)
                    dump(comb[:].rearrange("p t g e -> p (t g e)"), 320)
                    dump(oh[:].rearrange("p t g -> p (t g)"), 576)
                    dump(sel[:].rearrange("p t g -> p (t g)"), 640)
                    dump(ew[:].rearrange("p t g -> p (t g)"), 704)
                    dump(gw[:], 768)
                    for g4 in range(4):
                        ps = next_ps()
                        for i4 in range(4):
                            i = g4 * 4 + i4
                            S.transpose(ps[0:16, i4 * 128:(i4 + 1) * 128],
                                        comb[:, i, :, :].rearrange("p g e -> p (g e)"), ident)
                        S.copy('act', combT[0:16, g4 * 512:(g4 + 1) * 512], ps[0:16, :])
                    S.copy('dve', cT_hi[:], combT[:])
                    S.tt('dve', combT[:], combT[:], cT_hi[:], ALU.subtract)
                    S.copy('dve', cT_lo[:], combT[:])
                    barrier()
                    dump(combT[0:16, :], 1024)
                ps_set[0] = list(range(8))
                wg = sb(f"wg_{l}", [128, 2, NC8, EH], BF16, pc)
                wu = sb(f"wu_{l}", [128, 2, NC8, EH], BF16, pc)
                wd = sb(f"wd_{l}", [128, 2, 4, D], BF16, pc)
                sgs = sb(f"sgs_{l}", [128, 3, TT], F32, pc)
                tus = sb(f"tus_{l}", [128, 3, TT], F32, pc)
                hdn = sb(f"hdn_{l}", [128, 2, 4, TT], BF16, pc)

                def load_w(e):
                    b = e % 2
                    S.dma('pool', wg[:, b], w_gate[l, e].rearrange("(c p) n -> p c n", p=128))
                    S.dma('pool', wu[:, b], w_up[l, e].rearrange("(c p) n -> p c n", p=128))
                    S.dma('pool', wd[:, b], w_down[l, e].rearrange("(c p) n -> p c n", p=128))

                units = [(e, tt) for e in range(NE) for tt in range(NTT)]
                rr = [0]

                def gu(u, ui):
                    e, tt = u
                    b = e % 2
                    pcb = PS[6 + ui % 2]
                    S.mm(pcb[:], sel16[:, e, :], cT_hi[:, tts(tt)], True, False)
                    S.mm(pcb[:], sel16[:, e, :], cT_lo[:, tts(tt)], False, True)
                    hb = hdn[:, ui % 2]
                    for hc in range(4):
                        pg = next_ps()
                        pu = next_ps()
                        for k in range(NC8):
                            S.mm(pg[:], wg[:, b, k, hc * 128:(hc + 1) * 128], xn(k, tt * TT, TT), k == 0, k == NC8 - 1)
                        for k in range(NC8):
                            S.mm(pu[:], wu[:, b, k, hc * 128:(hc + 1) * 128], xn(k, tt * TT, TT), k == 0, k == NC8 - 1)
                        r = rr[0] % 3
                        rr[0] += 1
                        S.act(sgs[:, r, :], pg[:], AF.Silu)
                        S.tt('dve', tus[:, r, :], pu[:], sgs[:, r, :], ALU.mult)
                        S.tt('dve', hb[:, hc, :], tus[:, r, :], pcb[:], ALU.mult)
                        if False:
                            dbt = sb("dbt", [128, 4, TT], F32, pc)
                            S.copy('dve', dbt[:, 0, :], pcb[:])
                            S.copy('act', dbt[:, 1, :], pg[:])
                            S.copy('dve', dbt[:, 2, :], pu[:])
                            S.copy('dve', dbt[:, 3, :], hb[:, hc, :])
                            dump(dbt[:].rearrange("p a b -> p (a b)"), 4096)

                def down(u, ui):
                    e, tt = u
                    b = e % 2
                    hb = hdn[:, ui % 2]
                    for dc in range(NC8):
                        pd = next_ps()
                        for hc in range(4):
                            S.mm(pd[:], wd[:, b, hc, dc * 128:(dc + 1) * 128], hb[:, hc, :], hc == 0, hc == 3)
                        if dbg is not None and ui == 5 and dc == 2:
                            dbt2 = sb("dbt2", [128, 1, TT], F32, pc)
                            S.copy('dve', dbt2[:, 0, :], pd[:])
                            dump(dbt2[:].rearrange("p a b -> p (a b)"), 4096)
                            dbt3 = sb("dbt3", [128, 4, TT], BF16, pc)
                            for hc in range(4):
                                S.copy('dve', dbt3[:, hc, :], hb[:, hc, :])
                            dump(dbt3[:].rearrange("p a b -> p (a b)").bitcast(F32), 4608)
                        S.tt('dve', h[:, dc, tts(tt)], pd[:], h[:, dc, tts(tt)], ALU.add)

                load_w(0)
                ps_set[0] = list(range(6))
                for ui, u in enumerate(units):
                    e, tt = u
                    if tt == 0 and e + 1 < NE:
                        pass
                    gu(u, ui)
                    if DBG_BARRIER:
                        barrier()
                    if ui > 0:
                        down(units[ui - 1], ui - 1)
                    if DBG_BARRIER:
                        barrier()
                    if tt == 0 and e + 1 < NE:
                        load_w(e + 1)
                down(units[-1], len(units) - 1)
                ps_set[0] = list(range(8))
            barrier()

        def phase_C(l, early_out=None):
            with ExitStack() as pc:
                wpg = sb(f"wpg_{l}", [128, NC8, D], BF16, pc)
                wpp = sb(f"wpp_{l}", [128, 2, D], BF16, pc)
                ptb = sb(f"ptb_{l}", [128, 2, S_LEN], BF16, pc)
                sgc = sb(f"sgc_{l}", [128, 3, TT], F32, pc)
                tc_ = sb(f"tc_{l}", [128, 3, TT], F32, pc)
                wv = w_pg[l].rearrange("(c p) n -> p c n", p=128)
                for c in range(NC8):
                    S.dma('pool', wpg[:, c, :], wv[:, c, :])
                S.dma('pool', wpp[:], w_pp[l].rearrange("(c p) n -> p c n", p=128))
                pv = pT[l].rearrange("(c p) t -> p c t", p=128)
                for c in range(2):
                    S.dma('pool', ptb[:, c, :], pv[:, c, :])
                rms_tile(0, 'g_ple', l, lambda c: xn(c, 0, TT))
                r = 0
                for tt in range(NTT):
                    if tt + 1 < NTT:
                        rms_tile(tt + 1, 'g_ple', l, lambda c, t1=tt + 1: xn(c, t1 * TT, TT))
                    for co in range(NC8):
                        pg = next_ps()
                        pp = next_ps()
                        for k in range(NC8):
                            S.mm(pg[:], wpg[:, k, co * 128:(co + 1) * 128], xn(k, tt * TT, TT), k == 0, k == NC8 - 1)
                        for k in range(2):
                            S.mm(pp[:], wpp[:, k, co * 128:(co + 1) * 128], ptb[:, k, tts(tt)], k == 0, k == 1)
                        S.act(sgc[:, r % 3, :], pg[:], AF.Sigmoid)
                        S.tt('dve', tc_[:, r % 3, :], pp[:], sgc[:, r % 3, :], ALU.mult)
                        S.tt('pool', h[:, co, tts(tt)], h[:, co, tts(tt)], tc_[:, r % 3, :], ALU.add)
                        if early_out is not None:
                            early_out(co, tt)
                        r += 1
            barrier()

        def phase_D():
            SCALE = HD ** -0.5
            with ExitStack() as pc:
                ao = sb("ao", [128, NH, S_LEN], BF16, pc)
                wh = sb("wh", [128, 2, 3, NC8, HD], BF16, pc)
                qT_ = sb("qT", [128, S_LEN], BF16, pc)
                kT_ = sb("kT", [128, S_LEN], BF16, pc)
                vh = sb("vh", [128, 16, HD], BF16, pc)
                q32 = sb("q32", [128, 2, TT], F32, pc)
                ksum = sb("ksum", [128, 8], F32, pc)
                gs = sb("gs", [128, 8, 8], F32, pc)
                top8 = sb("top8", [128, 8, 8], F32, pc)
                ext = sb("ext", [128, 16, 40], F32, pc)
                rh = sb("rh", [128, S_LEN], BF16, pc)
                rtD1 = sb("rtD1", [128, TT], F32, pc)
                rsD1 = sb("rsD1", [128, TT], F32, pc)
                rtD = [rt, rtD1]
                rsD = [rstd, rsD1]
                pt_ = sb("pt", [128, 4, 256], BF16, pc)
                rden = sb("rden", [128, 2, 256], F32, pc)
                wo = sb("wo", [128, NC8, D], BF16, pc)
                rms_tile(0, 'g_mix', 1, lambda c: xn(c, 0, TT))
                rms_tile(1, 'g_mix', 1, lambda c: xn(c, TT, TT))
                rms_pending = [2, 3]
                S.memset('pool', ext[:], 0.0)
                S.memset('pool', rh[:], 0.0)
                wqv = w_qkv.rearrange("(c p) n -> p c n", p=128)

                def load_head(hh):
                    b = hh % 2
                    for j in range(3):
                        S.dma('pool', wh[:, b, j], wqv[:, :, j * D + hh * HD:j * D + (hh + 1) * HD])

                load_head(0)
                wov = w_o.rearrange("(c p) n -> p c n", p=128)
                for c in range(NC8):
                    S.dma('pool', wo[:, c, :], wov[:, c, :])
                gen = [0, 1, 2]
                for hh in range(NH):
                    b = hh % 2
                    if hh + 1 < NH:
                        load_head(hh + 1)
                    ps_set[0] = [0, 1, 2, 4, 5, 6, 7]
                    psg = PS[3]
                    tiles = [(1, tt) for tt in range(NTT)] + [(0, tt) for tt in range(NTT)]
                    pst = {}

                    def stage1(n):
                        j, tt = tiles[n]
                        ps = next_ps()
                        for k in range(NC8):
                            S.mm(ps[:], wh[:, b, j, k, :], xn(k, tt * TT, TT), k == 0, k == NC8 - 1)
                        S.act(sq[:, n % 2, :], ps[:], AF.Square)
                        p2_ = next_ps()
                        S.mm(p2_[:], ones_bf[:], sq[:, n % 2, :], True, True)
                        S.act(rtD[n % 2][:], p2_[:], AF.Ln, bias=EPS, scale=1.0 / HD)
                        S.act(rsD[n % 2][:], rtD[n % 2][:], AF.Exp, scale=-0.5)
                        pst[n] = ps

                    def stage2(n):
                        j, tt = tiles[n]
                        ps = pst[n]
                        d32 = q32[:, n % 2, :]
                        if j == 1:
                            S.stt(d32, ps[:], P('k_gain'), rsD[n % 2][:], ALU.mult, ALU.mult)
                            S.copy('act', kT_[:, tts(tt)], d32)
                            S.reduce(ksum[:, 2 * tt:2 * tt + 2], d32.rearrange("p (a b) -> p a b", b=256), ALU.add)
                        else:
                            S.stt(d32, ps[:], P('q_gain'), rsD[n % 2][:], ALU.mult, ALU.mult)
                            S.act(qT_[:, tts(tt)], d32, AF.Copy, scale=SCALE)
                            if tt >= 2:
                                for i4 in range(4):
                                    i8 = tt * 4 + i4 - 8
                                    S.mm(psg[:, i8 * 8:(i8 + 1) * 8], d32[:, i4 * 128:(i4 + 1) * 128], ksum[:], True, True)

                    stage1(0)
                    for n in range(len(tiles)):
                        if n + 1 < len(tiles):
                            stage1(n + 1)
                        stage2(n)
                        while n == 0 and rms_pending:
                            t1 = rms_pending.pop(0)
                            rms_tile(t1, 'g_mix', 1, lambda c, t1=t1: xn(c, t1 * TT, TT))
                    ps_set[0] = gen
                    S.tt('dve', gs[:], psg[:, 0:64].rearrange("p (a b) -> p a b", b=8),
                         cst[:, CO['negm']:CO['negm'] + 64].rearrange("p (a b) -> p a b", b=8), ALU.add)
                    for i8 in range(8):
                        S.op('dve', lambda e, i8=i8: e.max(out=top8[:, i8, :], in_=gs[:, i8, :]),
                             ins=[gs[:, i8, :]], outs=[top8[:, i8, :]])
                    for i8 in range(8):
                        S.ts('dve', ext[:, 8 + i8, 32:40], gs[:, i8, :], top8[:, i8, 2:3], ALU.is_lt, -30000.0, ALU.mult)
                    S.copy('dve', ext[:, :, 0].rearrange("p (a b) -> p a b", b=2),
                           cst[:, CO['alc'] + hh * 2:CO['alc'] + hh * 2 + 2].unsqueeze(1).to_broadcast([128, 8, 2]))
                    for g4 in range(4):
                        ps = next_ps()
                        for i4 in range(4):
                            i = g4 * 4 + i4
                            S.transpose(ps[0:40, i4 * 128:(i4 + 1) * 128], ext[:, i, :], ident)
                        S.copy('act', rh[0:40, g4 * 512:(g4 + 1) * 512], ps[0:40, :])
                    for g4 in range(4):
                        ps = next_ps()
                        for i4 in range(4):
                            i = g4 * 4 + i4
                            for k in range(NC8):
                                S.mm(ps[:, i4 * 128:(i4 + 1) * 128], xn(k, i * 128, 128), wh[:, b, 2, k, :],
                                     k == 0, k == NC8 - 1)
                        S.copy('act', vh[:, g4 * 4:(g4 + 1) * 4, :].rearrange("p a b -> p (a b)"), ps[:])
                    prr = [0]
                    for jq in range(8):
                        po = PS[4 + jq % 2]
                        pd = PS[6 + jq % 2]
                        kts = list(range(2 * jq + 2))
                        info = []
                        for kt in kts:
                            n = kt // 2
                            own = (n == jq)
                            second = own and (kt == 2 * jq + 1)
                            c0 = 128 if second else 0
                            nq = 128 if second else 256
                            ssel = 8 if (own or jq <= 3) else n
                            info.append((kt, own, c0, nq, ssel))
                        stile = {}

                        def emit_s(idx):
                            kt, own, c0, nq, ssel = info[idx]
                            ps = next_ps()
                            q0 = jq * 256 + c0
                            S.mm(ps[:, 0:nq], kT_[:, kt * 128:(kt + 1) * 128], qT_[:, q0:q0 + nq], True, False)
                            S.mm(ps[:, 0:nq], esel_bf[:, ssel, :], rh[:, q0:q0 + nq], False, True)
                            pb = pt_[:, prr[0] % 4, :]
                            prr[0] += 1
                            dlt = 2 * jq - kt + 1
                            S.act(pb[:, 0:nq], ps[:, 0:nq], AF.Exp,
                                  bias=cst[:, CO['ab'] + hh * 17 + dlt:CO['ab'] + hh * 17 + dlt + 1])
                            if own:
                                S.tt('pool', pb[:, 0:128], pb[:, 0:128], tri_bf[:], ALU.mult)
                            stile[idx] = pb

                        def emit_pv(idx):
                            kt, own, c0, nq, ssel = info[idx]
                            pb = stile[idx]
                            first = idx == 0
                            last = idx == len(info) - 1
                            S.mm(po[:, c0:c0 + nq], vh[:, kt, :], pb[:, 0:nq], first, last, inc=last)
                            S.mm(pd[:, c0:c0 + nq], ones_bf[:], pb[:, 0:nq], first, last, inc=True)

                        LA = 2
                        for idx in range(min(LA, len(info))):
                            emit_s(idx)
                        for idx in range(len(info)):
                            if idx + LA < len(info):
                                emit_s(idx + LA)
                            emit_pv(idx)
                        S.act(rden[:, jq % 2, :], pd[:, 0:256], AF.Ln)
                        S.act(rden[:, jq % 2, :], rden[:, jq % 2, :], AF.Exp, scale=-1.0)
                        S.tt('dve', ao[:, hh, jq * 256:(jq + 1) * 256], po[:, 0:256], rden[:, jq % 2, :], ALU.mult)
                ps_set[0] = list(range(8))
                for tt in range(NTT):
                    for co in range(NC8):
                        ps = next_ps()
                        for k in range(NC8):
                            S.mm(ps[:], wo[:, k, co * 128:(co + 1) * 128], ao[:, k, tts(tt)], k == 0, k == NC8 - 1)
                        S.tt('dve', h[:, co, tts(tt)], ps[:], h[:, co, tts(tt)], ALU.add)
            barrier()

        yv = yT.rearrange("(c p) t -> p c t", p=128)
        out_toks = []
        stored = [False]
        barrier()
        for ph in phases:
            if ph == 'A':
                phase_A()
            elif ph == 'B0':
                phase_B(0)
            elif ph == 'B1':
                phase_B(1)
            elif ph == 'C0':
                phase_C(0)
            elif ph == 'C1':
                if ph == phases[-1] and dbg is None:
                    phase_C(1, early_out=lambda co, tt: out_toks.append(
                        S.dma('sp', yv[:, co, tt * TT:(tt + 1) * TT], h[:, co, tt * TT:(tt + 1) * TT])))
                    stored[0] = True
                else:
                    phase_C(1)
            elif ph == 'D':
                phase_D()

        toks = list(out_toks)
        if not stored[0]:
            for c in range(NC8):
                toks.append(S.dma('sp' if c % 2 == 0 else 'act', yv[:, c, :], h[:, c, :]))
        for t in toks + dbg_toks:
            S.wait_tok('sp', t)
        build_program.stats = (S.nops, S.nwaits)
    return nc


_CACHE = {}


LAST = {}


def run(inputs, phases=('A', 'B0', 'C0', 'D', 'B1', 'C1'), debug=False):
    key = (tuple(phases), debug)
    if key not in _CACHE:
        _CACHE[key] = build_program(phases, debug)
    nc = _CACHE[key]
    x = np.asarray(inputs['x'], dtype=np.float32)
    p = np.asarray(inputs['p'], dtype=np.float32)
    prm = pack_params(inputs)
    cst = make_consts()
    shared = {
        "prm": prm, "cst": cst,
        "w_pw1": np.ascontiguousarray(np.asarray(inputs['conv_w_pw1'], dtype=np.float32)[0]),
        "w_pw2": np.ascontiguousarray(np.asarray(inputs['conv_w_pw2'], dtype=np.float32)[0]),
        "w_qkv": np.ascontiguousarray(np.asarray(inputs['attn_w_qkv'], dtype=np.float32)[0]),
        "w_o": np.ascontiguousarray(np.asarray(inputs['attn_w_o'], dtype=np.float32)[0]),
        "w_gate": np.ascontiguousarray(np.asarray(inputs['moe_w_gate'], dtype=np.float32)),
        "w_up": np.ascontiguousarray(np.asarray(inputs['moe_w_up'], dtype=np.float32)),
        "w_down": np.ascontiguousarray(np.asarray(inputs['moe_w_down'], dtype=np.float32)),
        "w_pg": np.ascontiguousarray(np.asarray(inputs['ple_w_gate'], dtype=np.float32)),
        "w_pp": np.ascontiguousarray(np.asarray(inputs['ple_w_proj'], dtype=np.float32)),
    }
    in_maps = []
    for b in range(NCORES):
        m = dict(shared)
        m["xT"] = np.ascontiguousarray(x[b].T)
        m["pT"] = np.ascontiguousarray(p[:, b].transpose(0, 2, 1))
        in_maps.append(m)
    res = run_bass_kernel_spmd(nc, in_maps, core_ids=list(range(NCORES)))
    if debug:
        LAST['dbg'] = [np.asarray(res.results[b]["dbg"]) for b in range(NCORES)]
    out = np.stack([np.asarray(res.results[b]["yT"], dtype=np.float32).T for b in range(NCORES)], axis=0)
    return np.ascontiguousarray(out)


def kernel(**inputs):
    return run(inputs)
```

```python
from contextlib import ExitStack

import numpy as np
import concourse.bass as bass
import concourse.mybir as mybir
from concourse.bass_utils import run_bass_kernel_spmd

F32 = mybir.dt.float32
BF16 = mybir.dt.bfloat16
AF = mybir.ActivationFunctionType
ALU = mybir.AluOpType
AX = mybir.AxisListType

D = 1024
S_LEN = 2048
NC8 = 8
NTT = 4
TT = 512
HALO = 30
NH = 8
HD = 128
NE = 16
EH = 512
PLE = 256
EPS = 1e-6
NCORES = 8


class Sched:
    def __init__(self, nc, ctx):
        self.nc = nc
        self.ctx = ctx
        self.eng = {'pe': nc.tensor, 'act': nc.scalar, 'dve': nc.vector, 'pool': nc.gpsimd, 'sp': nc.sync}
        self.sem = {}
        self.cnt = {}
        self.nsem = 0
        for e in ['pe', 'act', 'dve', 'pool']:
            self._new_sem(e)
        self.seen = {e: {} for e in self.eng}
        self.W = {}
        self.R = {}
        self.pend = {e: [] for e in self.eng}
        self.dma_sems = []
        self.dma_cnt = []
        self.dma_rr = {'hw': 0, 'sw': 0}
        self.dma_pool = {'hw': list(range(0, 8)), 'sw': list(range(8, 32))}
        for i in range(32):
            self.dma_sems.append(ctx.enter_context(nc.semaphore(f"dq{i}")))
            self.dma_cnt.append(0)
        self.nwaits = 0
        self.nops = 0

    def _new_sem(self, e):
        self.nsem += 1
        self.sem[e] = self.ctx.enter_context(self.nc.semaphore(f"s_{e}_{self.nsem}"))
        self.cnt[e] = 0

    @staticmethod
    def regions(ap):
        t = ap.tensor
        if str(ap.space) == 'DRAM':
            return []
        shape = t.shape
        row = 1
        for s in shape[1:]:
            row *= s
        off = int(ap.offset)
        dims = ap.ap
        p0 = off // row
        f0 = off % row
        npart = dims[0][1]
        fd = sorted([(abs(s), n) for (s, n) in dims[1:] if n > 1 and s != 0], reverse=True)

        def expand(ds, base, budget):
            if not ds:
                return [(base, base + 1)]
            s0, n0 = ds[0]
            inner = 1
            for (t_, m_) in ds[1:]:
                inner += (m_ - 1) * t_
            if s0 > inner and n0 <= budget:
                out = []
                for i in range(n0):
                    out += expand(ds[1:], base + i * s0, budget // n0)
                return out
            return [(base, base + inner + (n0 - 1) * s0)]

        name = t.name
        return [(name, p0, p0 + npart, a, b) for (a, b) in expand(fd, f0, 32)]

    @staticmethod
    def _ov(a, r):
        return a[0] < r[2] and r[1] < a[1] and a[2] < r[4] and r[3] < a[3]

    def _collect(self, engine, ins, outs):
        toks = []
        rin = [r for a in ins for r in self.regions(a)]
        rout = [r for a in outs for r in self.regions(a)]
        same = engine == 'pe'
        for r in rin:
            if r is None:
                continue
            for w in self.W.get(r[0], ()):
                if self._ov(w, r):
                    toks.append(w[4])
        for r in rout:
            if r is None:
                continue
            for w in self.W.get(r[0], ()):
                if self._ov(w, r) and not (same and w[4][2] == engine):
                    toks.append(w[4])
            for rd in self.R.get(r[0], ()):
                if self._ov(rd, r) and not (same and rd[4][2] == engine):
                    toks.append(rd[4])
        return toks, rin, rout

    def _wait(self, engine, toks):
        need = {}
        for (sem, val, src) in toks:
            k = id(sem)
            if k not in need or need[k][1] < val:
                need[k] = (sem, val, src)
        for k, (sem, val, src) in need.items():
            if src == engine and engine == 'pe':
                continue
            if self.seen[engine].get(k, 0) < val:
                self.eng[engine].wait_ge(sem, val)
                self.seen[engine][k] = val
                self.nwaits += 1

    def _record(self, tok, rin, rout):
        for r in rin:
            if r is None:
                continue
            lst = self.R.setdefault(r[0], [])
            lst[:] = [x for x in lst if not (x[4][0] is tok[0] and x[0] >= r[1] and x[1] <= r[2]
                                             and x[2] >= r[3] and x[3] <= r[4])]
            lst.append((r[1], r[2], r[3], r[4], tok))
        for r in rout:
            if r is None:
                continue
            lst = self.W.setdefault(r[0], [])
            lst[:] = [x for x in lst if not (x[0] >= r[1] and x[1] <= r[2] and x[2] >= r[3] and x[3] <= r[4])]
            lst.append((r[1], r[2], r[3], r[4], tok))
            rl = self.R.get(r[0])
            if rl:
                rl[:] = [x for x in rl if not (x[0] >= r[1] and x[1] <= r[2] and x[2] >= r[3] and x[3] <= r[4])]

    def op(self, engine, fn, ins=(), outs=(), inc=True):
        toks, rin, rout = self._collect(engine, ins, outs)
        self._wait(engine, toks)
        ins_obj = fn(self.eng[engine])
        self.nops += 1
        if inc:
            if self.cnt[engine] >= 30000:
                self._new_sem(engine)
            self.cnt[engine] += 1
            ins_obj.then_inc(self.sem[engine], 1)
            tok = (self.sem[engine], self.cnt[engine], engine)
            for (pi, po) in self.pend[engine]:
                self._record(tok, pi, po)
            self.pend[engine] = []
            self._record(tok, rin, rout)
            return tok
        else:
            self.pend[engine].append((rin, rout))
            return None

    def dma(self, queue, out, in_, **kw):
        kind = 'sw' if queue == 'pool' else 'hw'
        lst = self.dma_pool[kind]
        i = lst[self.dma_rr[kind] % len(lst)]
        self.dma_rr[kind] += 1
        sem = self.dma_sems[i]
        toks, rin, rout = self._collect('dma', [in_], [out])
        if self.dma_cnt[i] > 0:
            toks.append((sem, self.dma_cnt[i], 'dma'))
        self._wait(queue, toks)
        ins_obj = self.eng[queue].dma_start(out=out, in_=in_, **kw)
        self.dma_cnt[i] += 16
        ins_obj.then_inc(sem, 16)
        tok = (sem, self.dma_cnt[i], 'dma')
        self._record(tok, rin, rout)
        return tok

    def wait_tok(self, engine, tok):
        self._wait(engine, [tok])

    def mm(self, out, lhsT, rhs, start, stop, inc=None):
        inc = True
        return self.op('pe', lambda e: e.matmul(out, lhsT=lhsT, rhs=rhs, start=start, stop=stop),
                       ins=[lhsT, rhs], outs=[out], inc=inc)

    def transpose(self, out, in_, ident):
        return self.op('pe', lambda e: e.transpose(out, in_, ident), ins=[in_, ident], outs=[out])

    def act(self, out, in_, func, bias=None, scale=None, eng='act'):
        ins = [in_]
        kw = {}
        if bias is not None:
            kw['bias'] = bias
            if not isinstance(bias, (int, float)):
                ins.append(bias)
        if scale is not None:
            kw['scale'] = scale
            if not isinstance(scale, (int, float)):
                ins.append(scale)
        return self.op('act', lambda e: e.activation(out=out, in_=in_, func=func, **kw), ins=ins, outs=[out])

    def tt(self, eng, out, in0, in1, op):
        return self.op(eng, lambda e: e.tensor_tensor(out=out, in0=in0, in1=in1, op=op), ins=[in0, in1], outs=[out])

    def ts(self, eng, out, in0, s1, op0, s2=None, op1=None):
        ins = [in0]
        if not isinstance(s1, (int, float)):
            ins.append(s1)
        if s2 is not None and not isinstance(s2, (int, float)):
            ins.append(s2)
        if op1 is None:
            return self.op(eng, lambda e: e.tensor_scalar(out=out, in0=in0, scalar1=s1, scalar2=None, op0=op0),
                           ins=ins, outs=[out])
        return self.op(eng, lambda e: e.tensor_scalar(out=out, in0=in0, scalar1=s1, scalar2=s2, op0=op0, op1=op1),
                       ins=ins, outs=[out])

    def stt(self, out, in0, scalar, in1, op0, op1):
        ins = [in0, in1]
        if not isinstance(scalar, (int, float)):
            ins.append(scalar)
        return self.op('dve', lambda e: e.scalar_tensor_tensor(out=out, in0=in0, scalar=scalar, in1=in1, op0=op0, op1=op1),
                       ins=ins, outs=[out])

    def copy(self, eng, out, in_):
        if eng == 'act':
            return self.op('act', lambda e: e.copy(out=out, in_=in_), ins=[in_], outs=[out])
        return self.op(eng, lambda e: e.tensor_copy(out=out, in_=in_), ins=[in_], outs=[out])

    def recip(self, out, in_):
        return self.op('dve', lambda e: e.reciprocal(out=out, in_=in_), ins=[in_], outs=[out])

    def reduce(self, out, in_, op):
        return self.op('dve', lambda e: e.tensor_reduce(out=out, in_=in_, axis=AX.X, op=op), ins=[in_], outs=[out])

    def memset(self, eng, ap, val):
        return self.op(eng, lambda e: e.memset(ap, val), ins=[], outs=[ap])


class Packer:
    def __init__(self):
        self.cols = []
        self.off = {}
        self.n = 0

    def add(self, name, arr):
        arr = np.ascontiguousarray(arr, dtype=np.float32)
        assert arr.shape[0] == 128, (name, arr.shape)
        arr = arr.reshape(128, -1)
        self.off[name] = self.n
        self.n += arr.shape[1]
        self.cols.append(arr)

    def build(self):
        return np.ascontiguousarray(np.concatenate(self.cols, axis=1))


def feat_cols(v):
    v = np.asarray(v, dtype=np.float32)
    return v.reshape(-1, 128).T


def prm_layout():
    off = {}
    n = 0
    for name, w in [('g_mix', 16), ('g_ffn', 16), ('g_ple', 16), ('b_pw1', 16), ('w_dw', 248), ('b_dw', 8),
                    ('ln_g', 8), ('ln_b', 8), ('b_pw2', 8), ('q_gain', 1), ('k_gain', 1),
                    ('wr0', 160), ('wr1', 160), ('rb0', 20), ('rb1', 20)]:
        off[name] = n
        n += w
    return off, n


def pack_params(inp):
    pk = Packer()
    pk.add('g_mix', feat_cols(inp['g_mix']))
    pk.add('g_ffn', feat_cols(inp['g_ffn']))
    pk.add('g_ple', feat_cols(inp['g_ple']))
    pk.add('b_pw1', feat_cols(inp['conv_b_pw1'][0]))
    wdw = np.asarray(inp['conv_w_dw'][0], dtype=np.float32)
    pk.add('w_dw', wdw.reshape(31, 8, 128).transpose(2, 1, 0).reshape(128, 248))
    pk.add('b_dw', feat_cols(inp['conv_b_dw'][0]))
    pk.add('ln_g', feat_cols(inp['conv_ln_g'][0]))
    pk.add('ln_b', feat_cols(inp['conv_ln_b'][0]))
    pk.add('b_pw2', feat_cols(inp['conv_b_pw2'][0]))
    pk.add('q_gain', np.asarray(inp['attn_q_gain'][0], dtype=np.float32).reshape(128, 1))
    pk.add('k_gain', np.asarray(inp['attn_k_gain'][0], dtype=np.float32).reshape(128, 1))
    for l in range(2):
        wg = np.asarray(inp['moe_w_group'][l], dtype=np.float32)
        wr = np.asarray(inp['moe_w_router'][l], dtype=np.float32)
        wall = np.concatenate([wg, wr.transpose(1, 0, 2).reshape(1024, 16)], axis=1)
        pk.add(f'wr{l}', wall.reshape(8, 128, 20).transpose(1, 0, 2).reshape(128, 160))
    for l in range(2):
        b = np.concatenate([np.asarray(inp['moe_b_group'][l], dtype=np.float32).reshape(4),
                            np.asarray(inp['moe_b_router'][l], dtype=np.float32).reshape(16)])
        pk.add(f'rb{l}', np.tile(b[None, :], (128, 1)))
    arr = pk.build()
    off, n = prm_layout()
    assert off == pk.off and n == arr.shape[1]
    return arr


def cst_layout():
    off = {}
    n = 0
    for name, w in [('ident', 128), ('tri', 128), ('ab', 8 * 17), ('alc', 16), ('negm', 64), ('esel', 9 * 128)]:
        off[name] = n
        n += w
    return off, n


def make_consts():
    pk = Packer()
    pk.add('ident', np.eye(128, dtype=np.float32))
    p = np.arange(128)[:, None]
    f = np.arange(128)[None, :]
    pk.add('tri', (f >= p).astype(np.float32))
    slopes = np.array([2.0 ** (-(i + 1)) for i in range(8)], dtype=np.float64)
    ab = np.zeros((128, 8, 17), dtype=np.float64)
    for h in range(8):
        for d in range(17):
            ab[:, h, d] = slopes[h] * (np.arange(128) - 128.0 * (d - 1))
    pk.add('ab', ab.reshape(128, 136))
    alc = np.zeros((128, 8, 2), dtype=np.float64)
    for h in range(8):
        for par in range(2):
            alc[:, h, par] = -slopes[h] * (par * 128 + np.arange(128))
    pk.add('alc', alc.reshape(128, 16))
    negm = np.zeros((128, 8, 8), dtype=np.float32)
    for i8 in range(8):
        j = (8 + i8) // 2
        for n in range(8):
            if n >= j:
                negm[:, i8, n] = -1e30
    pk.add('negm', negm.reshape(128, 64))
    esel = np.zeros((128, 9, 128), dtype=np.float32)
    for s in range(9):
        esel[0, s, :] = 1.0
        if s < 8:
            esel[32 + s, s, :] = 1.0
    pk.add('esel', esel.reshape(128, 9 * 128))
    arr = pk.build()
    off, n = cst_layout()
    assert off == pk.off and n == arr.shape[1]
    return arr


def build_program(phases=('A', 'B0', 'C0', 'D', 'B1', 'C1'), debug=False):
    nc = bass.Bass("TRN2", target_bir_lowering=False)
    PO, NPRM = prm_layout()
    CO, NCST = cst_layout()

    def din(name, shape):
        return nc.dram_tensor(name, list(shape), F32, kind="ExternalInput").ap()

    xT = din("xT", [D, S_LEN])
    pT = din("pT", [2, PLE, S_LEN])
    prm_d = din("prm", [128, NPRM])
    cst_d = din("cst", [128, NCST])
    w_pw1 = din("w_pw1", [D, 2 * D])
    w_pw2 = din("w_pw2", [D, D])
    w_qkv = din("w_qkv", [D, 3 * D])
    w_o = din("w_o", [D, D])
    w_gate = din("w_gate", [2, NE, D, EH])
    w_up = din("w_up", [2, NE, D, EH])
    w_down = din("w_down", [2, NE, EH, D])
    w_pg = din("w_pg", [2, D, D])
    w_pp = din("w_pp", [2, PLE, D])
    yT = nc.dram_tensor("yT", [D, S_LEN], F32, kind="ExternalOutput").ap()
    dbg = nc.dram_tensor("dbg", [128, 8192], F32, kind="ExternalOutput").ap() if debug else None

    with ExitStack() as ctx:
        S = Sched(nc, ctx)

        def sb(name, shape, dt, c=ctx):
            return c.enter_context(nc.sbuf_tensor(name, list(shape), dt))

        h = sb("h", [128, NC8, S_LEN], F32)
        xg = sb("xg", [128, NC8, HALO + S_LEN], BF16)
        prm = sb("prm_s", [128, NPRM], F32)
        cst = sb("cst_s", [128, NCST], F32)
        ones_bf = sb("ones_bf", [128, 128], BF16)
        tri_bf = sb("tri_bf", [128, 128], BF16)
        esel_bf = sb("esel_bf", [128, 9, 128], BF16)
        sq = sb("sq", [128, 2, TT], BF16)
        rt = sb("rt", [128, TT], F32)
        rstd = sb("rstd", [128, TT], F32)
        PS = [ctx.enter_context(nc.psum_tensor(f"ps{i}", [128, 512], F32)) for i in range(8)]
        ps_rr = [0]
        ps_set = [list(range(8))]

        def next_ps():
            lst = ps_set[0]
            b = lst[ps_rr[0] % len(lst)]
            ps_rr[0] += 1
            return PS[b]

        ident = cst[:, CO['ident']:CO['ident'] + 128]

        def xn(c, lo, n):
            return xg[:, c, HALO + lo:HALO + lo + n]

        def P(name, j=0, n=1):
            return prm[:, PO[name] + j:PO[name] + j + n]

        S.dma('sp', cst[:], cst_d)
        S.dma('sp', prm[:], prm_d)
        xv = xT.rearrange("(c p) t -> p c t", p=128)
        for tt0 in range(NTT):
            for c in range(NC8):
                S.dma('sp' if c % 2 == 0 else 'act', h[:, c, tt0 * TT:(tt0 + 1) * TT], xv[:, c, tt0 * TT:(tt0 + 1) * TT])
        S.memset('dve', ones_bf[:], 1.0)
        S.copy('dve', tri_bf[:], cst[:, CO['tri']:CO['tri'] + 128])
        S.copy('dve', esel_bf[:].rearrange("p a b -> p (a b)"), cst[:, CO['esel']:CO['esel'] + 9 * 128])
        S.memset('pool', xg[:, :, 0:HALO], 0.0)

        def tts(tt):
            return slice(tt * TT, (tt + 1) * TT)

        dbg_toks = []

        def dump(ap, col):
            if dbg is None:
                return
            np_, n = ap.shape[0], ap.shape[1]
            dbg_toks.append(S.dma('sp', dbg[0:np_, col:col + n], ap))

        def rms_tile(tt, gname, gl, out_fn):
            ps = next_ps()
            for c in range(NC8):
                S.act(sq[:, c % 2, :], h[:, c, tts(tt)], AF.Square)
                S.mm(ps[:], ones_bf[:], sq[:, c % 2, :], start=(c == 0), stop=(c == NC8 - 1))
            S.act(rt[:], ps[:], AF.Ln, bias=EPS, scale=1.0 / D)
            S.act(rstd[:], rt[:], AF.Exp, scale=-0.5)
            for c in range(NC8):
                S.stt(out_fn(c), h[:, c, tts(tt)], P(gname, gl * 8 + c), rstd[:], ALU.mult, ALU.mult)

        def barrier():
            toks = []
            for e in ['pe', 'act', 'dve', 'pool']:
                if S.cnt[e] > 0:
                    toks.append((S.sem[e], S.cnt[e], e))
            for i, sem in enumerate(S.dma_sems):
                if S.dma_cnt[i] > 0:
                    toks.append((sem, S.dma_cnt[i], 'dma'))
            for e in ['pe', 'act', 'dve', 'pool', 'sp']:
                S._wait(e, toks)

        def phase_A():
            with ExitStack() as pc:
                rms_tile(0, 'g_mix', 0, lambda c: xn(c, 0, TT))
                glu = sb("glu", [128, NC8, HALO + S_LEN], BF16, pc)
                S.memset('pool', glu[:, :, 0:HALO], 0.0)
                with ExitStack() as p2:
                    w1 = sb("w1", [128, NC8, 2 * D], BF16, p2)
                    sg = sb("sgA", [128, 2, TT], F32, p2)
                    w1v = w_pw1.rearrange("(c p) n -> p c n", p=128)
                    for c in range(NC8):
                        S.dma('pool', w1[:, c, :], w1v[:, c, :])
                    for tt in range(NTT):
                        if tt + 1 < NTT:
                            rms_tile(tt + 1, 'g_mix', 0, lambda c, t1=tt + 1: xn(c, t1 * TT, TT))
                        for c in range(NC8):
                            pa = next_ps()
                            pg = next_ps()
                            for k in range(NC8):
                                S.mm(pa[:], w1[:, k, c * 128:(c + 1) * 128], xn(k, tt * TT, TT), k == 0, k == NC8 - 1)
                            for k in range(NC8):
                                S.mm(pg[:], w1[:, k, D + c * 128:D + (c + 1) * 128], xn(k, tt * TT, TT), k == 0, k == NC8 - 1)
                            S.act(sg[:, c % 2, :], pg[:], AF.Sigmoid, bias=P('b_pw1', 8 + c))
                            S.stt(glu[:, c, HALO + tt * TT:HALO + (tt + 1) * TT], pa[:], P('b_pw1', c), sg[:, c % 2, :],
                                  ALU.add, ALU.mult)
                    barrier()
                dg = sb("dg", [128, 2, 31, 128], BF16, pc)
                w2 = sb("w2", [128, NC8, D], BF16, pc)
                w2v = w_pw2.rearrange("(c p) n -> p c n", p=128)
                for c in range(NC8):
                    S.dma('pool', w2[:, c, :], w2v[:, c, :])

                def cvb(c, tt):
                    return xn(c, tt * TT, TT)

                for c in range(NC8):
                    wd = prm[:, PO['w_dw'] + c * 31:PO['w_dw'] + (c + 1) * 31]
                    S.tt('dve', dg[:, c % 2], ident.unsqueeze(1).to_broadcast([128, 31, 128]),
                         wd.unsqueeze(2).to_broadcast([128, 31, 128]), ALU.mult)
                    for tt in range(NTT):
                        ps = next_ps()
                        for j in range(31):
                            S.mm(ps[:], dg[:, c % 2, j, :], glu[:, c, tt * TT + j:tt * TT + j + TT], j == 0, j == 30)
                        S.act(cvb(c, tt), ps[:], AF.Identity, bias=P('b_dw', c))
                mean = sb("meanA", [128, TT], F32, pc)
                m2 = sb("m2A", [128, TT], F32, pc)
                var = sb("varA", [128, TT], F32, pc)
                t1 = sb("t1A", [128, 2, TT], F32, pc)
                t2 = sb("t2A", [128, 2, TT], F32, pc)
                for tt in range(NTT):
                    p1 = next_ps()
                    p2_ = next_ps()
                    for c in range(NC8):
                        S.act(sq[:, c % 2, :], cvb(c, tt), AF.Square)
                        S.mm(p2_[:], ones_bf[:], sq[:, c % 2, :], c == 0, c == NC8 - 1)
                    for c in range(NC8):
                        S.mm(p1[:], ones_bf[:], cvb(c, tt), c == 0, c == NC8 - 1)
                    S.ts('dve', mean[:], p1[:], 1.0 / D, ALU.mult)
                    S.tt('dve', m2[:], mean[:], mean[:], ALU.mult)
                    S.stt(var[:], p2_[:], 1.0 / D, m2[:], ALU.mult, ALU.subtract)
                    S.act(rt[:], var[:], AF.Ln, bias=EPS)
                    S.act(rstd[:], rt[:], AF.Exp, scale=-0.5)
                    for c in range(NC8):
                        S.tt('pool', t1[:, c % 2, :], cvb(c, tt), mean[:], ALU.subtract)
                        S.tt('dve', t2[:, c % 2, :], t1[:, c % 2, :], rstd[:], ALU.mult)
                        S.act(xn(c, tt * TT, TT), t2[:, c % 2, :], AF.Silu, bias=P('ln_b', c), scale=P('ln_g', c))
                for tt in range(NTT):
                    for co in range(NC8):
                        ps = next_ps()
                        for k in range(NC8):
                            S.mm(ps[:], w2[:, k, co * 128:(co + 1) * 128], xn(k, tt * TT, TT), k == 0, k == NC8 - 1)
                        S.stt(h[:, co, tts(tt)], ps[:], P('b_pw2', co), h[:, co, tts(tt)], ALU.add, ALU.add)
            barrier()

        def phase_B(l):
            with ExitStack() as pc:
                cT_hi = sb(f"cThi_{l}", [128, S_LEN], BF16, pc)
                cT_lo = sb(f"cTlo_{l}", [128, S_LEN], BF16, pc)
                sel16 = sb(f"sel16_{l}", [128, 16, 128], BF16, pc)
                S.copy('dve', sel16[:], cst[:, CO['ident']:CO['ident'] + 16].unsqueeze(2).to_broadcast([128, 16, 128]))
                wr = prm[:, PO[f'wr{l}']:PO[f'wr{l}'] + 160]
                psL = PS[7]
                ps_set[0] = list(range(7))
                rc = ExitStack()
                combT = sb(f"combT_{l}", [128, S_LEN], F32, rc)
                S.memset('pool', combT[:], 0.0)
                x32 = sb(f"x32_{l}", [128, NC8, TT], F32, rc)
                lb = sb(f"lb_{l}", [128, 16, 20], F32, rc)
                lT = sb(f"lT_{l}", [128, 2, TT], F32, rc)
                for tt in range(NTT):
                    rms_tile(tt, 'g_ffn', l, lambda c: x32[:, c, :])
                    for c in range(NC8):
                        S.copy('act', xn(c, tt * TT, TT), x32[:, c, :])
                    pT_ = next_ps()
                    for c in range(NC8):
                        S.mm(pT_[0:20, :], wr[:, c * 20:(c + 1) * 20], x32[:, c, :], c == 0, c == NC8 - 1)
                    S.copy('act', lT[0:20, tt % 2, :], pT_[0:20, :])
                    for i4 in range(4):
                        i = tt * 4 + i4
                        S.transpose(psL[:, i * 20:(i + 1) * 20], lT[0:20, tt % 2, i4 * 128:(i4 + 1) * 128],
                                    cst[0:20, CO['ident']:CO['ident'] + 20])
                with rc:
                    def T(name, shape):
                        return sb(f"{name}_{l}", shape, F32, rc)
                    rb = prm[:, PO[f'rb{l}']:PO[f'rb{l}'] + 20]
                    S.tt('dve', lb[:], psL[:, 0:320].rearrange("p (t n) -> p t n", n=20),
                         rb.unsqueeze(1).to_broadcast([128, 16, 20]), ALU.add)
                    lg = lb[:, :, 0:4]
                    le = lb[:, :, 4:20].rearrange("p t (g e) -> p t g e", e=4)
                    m = T("r_m", [128, 16])
                    S.reduce(m[:], lg, ALU.max)
                    dd = T("r_d", [128, 16, 4])
                    S.tt('dve', dd[:], lg, m[:].unsqueeze(2).to_broadcast([128, 16, 4]), ALU.subtract)
                    oh = T("r_oh", [128, 16, 4])
                    S.ts('dve', oh[:], dd[:], 0.0, ALU.is_equal)
                    eg = T("r_eg", [128, 16, 4])
                    S.act(eg[:], dd[:], AF.Exp)
                    se = T("r_se", [128, 16])
                    S.reduce(se[:], eg[:], ALU.add)
                    gw = T("r_gw", [128, 16])
                    S.recip(gw[:], se[:])
                    tmp = T("r_tmp", [128, 16, 4, 4])
                    S.tt('dve', tmp[:], le, oh[:].unsqueeze(3).to_broadcast([128, 16, 4, 4]), ALU.mult)
                    sel = T("r_sel", [128, 16, 4])
                    S.reduce(sel[:], tmp[:].rearrange("p t g e -> p t e g"), ALU.add)
                    m1 = T("r_m1", [128, 16])
                    S.reduce(m1[:], sel[:], ALU.max)
                    eq1 = T("r_eq1", [128, 16, 4])
                    S.tt('dve', eq1[:], sel[:], m1[:].unsqueeze(2).to_broadcast([128, 16, 4]), ALU.is_equal)
                    sel2 = T("r_sel2", [128, 16, 4])
                    S.stt(sel2[:].rearrange("p t e -> p (t e)"), eq1[:].rearrange("p t e -> p (t e)"), -1e30,
                          sel[:].rearrange("p t e -> p (t e)"), ALU.mult, ALU.add)
                    m2_ = T("r_m2", [128, 16])
                    S.reduce(m2_[:], sel2[:], ALU.max)
                    eq2 = T("r_eq2", [128, 16, 4])
                    S.tt('dve', eq2[:], sel2[:], m2_[:].unsqueeze(2).to_broadcast([128, 16, 4]), ALU.is_equal)
                    d21 = T("r_d21", [128, 16])
                    S.tt('dve', d21[:], m2_[:], m1[:], ALU.subtract)
                    e21 = T("r_e21", [128, 16])
                    S.act(e21[:], d21[:], AF.Exp)
                    den = T("r_den", [128, 16])
                    S.ts('dve', den[:], e21[:], 1.0, ALU.add)
                    w1_ = T("r_w1", [128, 16])
                    S.recip(w1_[:], den[:])
                    w2_ = T("r_w2", [128, 16])
                    S.tt('dve', w2_[:], e21[:], w1_[:], ALU.mult)
                    w1g = T("r_w1g", [128, 16])
                    S.tt('dve', w1g[:], w1_[:], gw[:], ALU.mult)
                    w2g = T("r_w2g", [128, 16])
                    S.tt('dve', w2g[:], w2_[:], gw[:], ALU.mult)
                    ea = T("r_ea", [128, 16, 4])
                    S.tt('dve', ea[:], eq1[:], w1g[:].unsqueeze(2).to_broadcast([128, 16, 4]), ALU.mult)
                    eb = T("r_eb", [128, 16, 4])
                    S.tt('dve', eb[:], eq2[:], w2g[:].unsqueeze(2).to_broadcast([128, 16, 4]), ALU.mult)
                    ew = T("r_ew", [128, 16, 4])
                    S.tt('dve', ew[:], ea[:], eb[:], ALU.add)
                    comb = T("r_comb", [128, 16, 4, 4])
                    S.tt('dve', comb[:], oh[:].unsqueeze(3).to_broadcast([128, 16, 4, 4]),
                         ew[:].unsqueeze(2).to_broadcast([128, 16, 4, 4]), ALU.mult)
                    for g4 in range(4):
                        ps = next_ps()
                        for i4 in range(4):
                            i = g4 * 4 + i4
                            S.transpose(ps[0:16, i4 * 128:(i4 + 1) * 128],
                                        comb[:, i, :, :].rearrange("p g e -> p (g e)"), ident)
                        S.copy('act', combT[0:16, g4 * 512:(g4 + 1) * 512], ps[0:16, :])
                    S.copy('dve', cT_hi[:], combT[:])
                    S.tt('dve', combT[:], combT[:], cT_hi[:], ALU.subtract)
                    S.copy('dve', cT_lo[:], combT[:])
                    barrier()
                ps_set[0] = list(range(8))
                wg = sb(f"wg_{l}", [128, 2, NC8, EH], BF16, pc)
                wu = sb(f"wu_{l}", [128, 2, NC8, EH], BF16, pc)
                wd = sb(f"wd_{l}", [128, 2, 4, D], BF16, pc)
                sgs = sb(f"sgs_{l}", [128, 3, TT], F32, pc)
                tus = sb(f"tus_{l}", [128, 3, TT], F32, pc)
                hdn = sb(f"hdn_{l}", [128, 2, 4, TT], BF16, pc)

                def load_w(e):
                    b = e % 2
                    S.dma('pool', wg[:, b], w_gate[l, e].rearrange("(c p) n -> p c n", p=128))
                    S.dma('pool', wu[:, b], w_up[l, e].rearrange("(c p) n -> p c n", p=128))
                    S.dma('pool', wd[:, b], w_down[l, e].rearrange("(c p) n -> p c n", p=128))

                units = [(e, tt) for e in range(NE) for tt in range(NTT)]
                rr = [0]

                def gu(u, ui):
                    e, tt = u
                    b = e % 2
                    pcb = PS[6 + ui % 2]
                    S.mm(pcb[:], sel16[:, e, :], cT_hi[:, tts(tt)], True, False)
                    S.mm(pcb[:], sel16[:, e, :], cT_lo[:, tts(tt)], False, True)
                    hb = hdn[:, ui % 2]
                    for hc in range(4):
                        pg = next_ps()
                        pu = next_ps()
                        for k in range(NC8):
                            S.mm(pg[:], wg[:, b, k, hc * 128:(hc + 1) * 128], xn(k, tt * TT, TT), k == 0, k == NC8 - 1)
                        for k in range(NC8):
                            S.mm(pu[:], wu[:, b, k, hc * 128:(hc + 1) * 128], xn(k, tt * TT, TT), k == 0, k == NC8 - 1)
                        r = rr[0] % 3
                        rr[0] += 1
                        S.act(sgs[:, r, :], pg[:], AF.Silu)
                        S.tt('dve', tus[:, r, :], pu[:], sgs[:, r, :], ALU.mult)
                        S.tt('dve', hb[:, hc, :], tus[:, r, :], pcb[:], ALU.mult)

                def down(u, ui):
                    e, tt = u
                    b = e % 2
                    hb = hdn[:, ui % 2]
                    for dc in range(NC8):
                        pd = next_ps()
                        for hc in range(4):
                            S.mm(pd[:], wd[:, b, hc, dc * 128:(dc + 1) * 128], hb[:, hc, :], hc == 0, hc == 3)
                        S.tt('dve', h[:, dc, tts(tt)], pd[:], h[:, dc, tts(tt)], ALU.add)

                load_w(0)
                ps_set[0] = list(range(6))
                for ui, u in enumerate(units):
                    e, tt = u
                    if tt == 0 and e + 1 < NE:
                        pass
                    gu(u, ui)
                    if ui > 0:
                        down(units[ui - 1], ui - 1)
                    if tt == 0 and e + 1 < NE:
                        load_w(e + 1)
                down(units[-1], len(units) - 1)
                ps_set[0] = list(range(8))
            barrier()

        def phase_C(l, early_out=None):
            with ExitStack() as pc:
                wpg = sb(f"wpg_{l}", [128, NC8, D], BF16, pc)
                wpp = sb(f"wpp_{l}", [128, 2, D], BF16, pc)
                ptb = sb(f"ptb_{l}", [128, 2, S_LEN], BF16, pc)
                sgc = sb(f"sgc_{l}", [128, 3, TT], F32, pc)
                tc_ = sb(f"tc_{l}", [128, 3, TT], F32, pc)
                wv = w_pg[l].rearrange("(c p) n -> p c n", p=128)
                for c in range(NC8):
                    S.dma('pool', wpg[:, c, :], wv[:, c, :])
                S.dma('pool', wpp[:], w_pp[l].rearrange("(c p) n -> p c n", p=128))
                pv = pT[l].rearrange("(c p) t -> p c t", p=128)
                for c in range(2):
                    S.dma('pool', ptb[:, c, :], pv[:, c, :])
                rms_tile(0, 'g_ple', l, lambda c: xn(c, 0, TT))
                r = 0
                for tt in range(NTT):
                    if tt + 1 < NTT:
                        rms_tile(tt + 1, 'g_ple', l, lambda c, t1=tt + 1: xn(c, t1 * TT, TT))
                    for co in range(NC8):
                        pg = next_ps()
                        pp = next_ps()
                        for k in range(NC8):
                            S.mm(pg[:], wpg[:, k, co * 128:(co + 1) * 128], xn(k, tt * TT, TT), k == 0, k == NC8 - 1)
                        for k in range(2):
                            S.mm(pp[:], wpp[:, k, co * 128:(co + 1) * 128], ptb[:, k, tts(tt)], k == 0, k == 1)
                        S.act(sgc[:, r % 3, :], pg[:], AF.Sigmoid)
                        S.tt('dve', tc_[:, r % 3, :], pp[:], sgc[:, r % 3, :], ALU.mult)
                        S.tt('pool', h[:, co, tts(tt)], h[:, co, tts(tt)], tc_[:, r % 3, :], ALU.add)
                        if early_out is not None:
                            early_out(co, tt)
                        r += 1
            barrier()

        def phase_D():
            SCALE = HD ** -0.5
            with ExitStack() as pc:
                ao = sb("ao", [128, NH, S_LEN], BF16, pc)
                wh = sb("wh", [128, 2, 3, NC8, HD], BF16, pc)
                qT_ = sb("qT", [128, S_LEN], BF16, pc)
                kT_ = sb("kT", [128, S_LEN], BF16, pc)
                vh = sb("vh", [128, 16, HD], BF16, pc)
                q32 = sb("q32", [128, 2, TT], F32, pc)
                ksum = sb("ksum", [128, 8], F32, pc)
                gs = sb("gs", [128, 8, 8], F32, pc)
                top8 = sb("top8", [128, 8, 8], F32, pc)
                ext = sb("ext", [128, 16, 40], F32, pc)
                rh = sb("rh", [128, S_LEN], BF16, pc)
                rtD1 = sb("rtD1", [128, TT], F32, pc)
                rsD1 = sb("rsD1", [128, TT], F32, pc)
                rtD = [rt, rtD1]
                rsD = [rstd, rsD1]
                pt_ = sb("pt", [128, 4, 256], BF16, pc)
                rden = sb("rden", [128, 2, 256], F32, pc)
                wo = sb("wo", [128, NC8, D], BF16, pc)
                rms_tile(0, 'g_mix', 1, lambda c: xn(c, 0, TT))
                rms_tile(1, 'g_mix', 1, lambda c: xn(c, TT, TT))
                rms_pending = [2, 3]
                S.memset('pool', ext[:], 0.0)
                S.memset('pool', rh[:], 0.0)
                wqv = w_qkv.rearrange("(c p) n -> p c n", p=128)

                def load_head(hh):
                    b = hh % 2
                    for j in range(3):
                        S.dma('pool', wh[:, b, j], wqv[:, :, j * D + hh * HD:j * D + (hh + 1) * HD])

                load_head(0)
                wov = w_o.rearrange("(c p) n -> p c n", p=128)
                for c in range(NC8):
                    S.dma('pool', wo[:, c, :], wov[:, c, :])
                gen = [0, 1, 2]
                for hh in range(NH):
                    b = hh % 2
                    if hh + 1 < NH:
                        load_head(hh + 1)
                    ps_set[0] = [0, 1, 2, 4, 5, 6, 7]
                    psg = PS[3]
                    tiles = [(1, tt) for tt in range(NTT)] + [(0, tt) for tt in range(NTT)]
                    pst = {}

                    def stage1(n):
                        j, tt = tiles[n]
                        ps = next_ps()
                        for k in range(NC8):
                            S.mm(ps[:], wh[:, b, j, k, :], xn(k, tt * TT, TT), k == 0, k == NC8 - 1)
                        S.act(sq[:, n % 2, :], ps[:], AF.Square)
                        p2_ = next_ps()
                        S.mm(p2_[:], ones_bf[:], sq[:, n % 2, :], True, True)
                        S.act(rtD[n % 2][:], p2_[:], AF.Ln, bias=EPS, scale=1.0 / HD)
                        S.act(rsD[n % 2][:], rtD[n % 2][:], AF.Exp, scale=-0.5)
                        pst[n] = ps

                    def stage2(n):
                        j, tt = tiles[n]
                        ps = pst[n]
                        d32 = q32[:, n % 2, :]
                        if j == 1:
                            S.stt(d32, ps[:], P('k_gain'), rsD[n % 2][:], ALU.mult, ALU.mult)
                            S.copy('act', kT_[:, tts(tt)], d32)
                            S.reduce(ksum[:, 2 * tt:2 * tt + 2], d32.rearrange("p (a b) -> p a b", b=256), ALU.add)
                        else:
                            S.stt(d32, ps[:], P('q_gain'), rsD[n % 2][:], ALU.mult, ALU.mult)
                            S.act(qT_[:, tts(tt)], d32, AF.Copy, scale=SCALE)
                            if tt >= 2:
                                for i4 in range(4):
                                    i8 = tt * 4 + i4 - 8
                                    S.mm(psg[:, i8 * 8:(i8 + 1) * 8], d32[:, i4 * 128:(i4 + 1) * 128], ksum[:], True, True)

                    stage1(0)
                    for n in range(len(tiles)):
                        if n + 1 < len(tiles):
                            stage1(n + 1)
                        stage2(n)
                        while n == 0 and rms_pending:
                            t1 = rms_pending.pop(0)
                            rms_tile(t1, 'g_mix', 1, lambda c, t1=t1: xn(c, t1 * TT, TT))
                    ps_set[0] = gen
                    S.tt('dve', gs[:], psg[:, 0:64].rearrange("p (a b) -> p a b", b=8),
                         cst[:, CO['negm']:CO['negm'] + 64].rearrange("p (a b) -> p a b", b=8), ALU.add)
                    for i8 in range(8):
                        S.op('dve', lambda e, i8=i8: e.max(out=top8[:, i8, :], in_=gs[:, i8, :]),
                             ins=[gs[:, i8, :]], outs=[top8[:, i8, :]])
                    for i8 in range(8):
                        S.ts('dve', ext[:, 8 + i8, 32:40], gs[:, i8, :], top8[:, i8, 2:3], ALU.is_lt, -30000.0, ALU.mult)
                    S.copy('dve', ext[:, :, 0].rearrange("p (a b) -> p a b", b=2),
                           cst[:, CO['alc'] + hh * 2:CO['alc'] + hh * 2 + 2].unsqueeze(1).to_broadcast([128, 8, 2]))
                    for g4 in range(4):
                        ps = next_ps()
                        for i4 in range(4):
                            i = g4 * 4 + i4
                            S.transpose(ps[0:40, i4 * 128:(i4 + 1) * 128], ext[:, i, :], ident)
                        S.copy('act', rh[0:40, g4 * 512:(g4 + 1) * 512], ps[0:40, :])
                    for g4 in range(4):
                        ps = next_ps()
                        for i4 in range(4):
                            i = g4 * 4 + i4
                            for k in range(NC8):
                                S.mm(ps[:, i4 * 128:(i4 + 1) * 128], xn(k, i * 128, 128), wh[:, b, 2, k, :],
                                     k == 0, k == NC8 - 1)
                        S.copy('act', vh[:, g4 * 4:(g4 + 1) * 4, :].rearrange("p a b -> p (a b)"), ps[:])
                    prr = [0]
                    for jq in range(8):
                        po = PS[4 + jq % 2]
                        pd = PS[6 + jq % 2]
                        kts = list(range(2 * jq + 2))
                        info = []
                        for kt in kts:
                            n = kt // 2
                            own = (n == jq)
                            second = own and (kt == 2 * jq + 1)
                            c0 = 128 if second else 0
                            nq = 128 if second else 256
                            ssel = 8 if (own or jq <= 3) else n
                            info.append((kt, own, c0, nq, ssel))
                        stile = {}

                        def emit_s(idx):
                            kt, own, c0, nq, ssel = info[idx]
                            ps = next_ps()
                            q0 = jq * 256 + c0
                            S.mm(ps[:, 0:nq], kT_[:, kt * 128:(kt + 1) * 128], qT_[:, q0:q0 + nq], True, False)
                            S.mm(ps[:, 0:nq], esel_bf[:, ssel, :], rh[:, q0:q0 + nq], False, True)
                            pb = pt_[:, prr[0] % 4, :]
                            prr[0] += 1
                            dlt = 2 * jq - kt + 1
                            S.act(pb[:, 0:nq], ps[:, 0:nq], AF.Exp,
                                  bias=cst[:, CO['ab'] + hh * 17 + dlt:CO['ab'] + hh * 17 + dlt + 1])
                            if own:
                                S.tt('pool', pb[:, 0:128], pb[:, 0:128], tri_bf[:], ALU.mult)
                            stile[idx] = pb

                        def emit_pv(idx):
                            kt, own, c0, nq, ssel = info[idx]
                            pb = stile[idx]
                            first = idx == 0
                            last = idx == len(info) - 1
                            S.mm(po[:, c0:c0 + nq], vh[:, kt, :], pb[:, 0:nq], first, last, inc=last)
                            S.mm(pd[:, c0:c0 + nq], ones_bf[:], pb[:, 0:nq], first, last, inc=True)

                        LA = 2
                        for idx in range(min(LA, len(info))):
                            emit_s(idx)
                        for idx in range(len(info)):
                            if idx + LA < len(info):
                                emit_s(idx + LA)
                            emit_pv(idx)
                        S.act(rden[:, jq % 2, :], pd[:, 0:256], AF.Ln)
                        S.act(rden[:, jq % 2, :], rden[:, jq % 2, :], AF.Exp, scale=-1.0)
                        S.tt('dve', ao[:, hh, jq * 256:(jq + 1) * 256], po[:, 0:256], rden[:, jq % 2, :], ALU.mult)
                ps_set[0] = list(range(8))
                for tt in range(NTT):
                    for co in range(NC8):
                        ps = next_ps()
                        for k in range(NC8):
                            S.mm(ps[:], wo[:, k, co * 128:(co + 1) * 128], ao[:, k, tts(tt)], k == 0, k == NC8 - 1)
                        S.tt('dve', h[:, co, tts(tt)], ps[:], h[:, co, tts(tt)], ALU.add)
            barrier()

        yv = yT.rearrange("(c p) t -> p c t", p=128)
        out_toks = []
        stored = [False]
        barrier()
        for ph in phases:
            if ph == 'A':
                phase_A()
            elif ph == 'B0':
                phase_B(0)
            elif ph == 'B1':
                phase_B(1)
            elif ph == 'C0':
                phase_C(0)
            elif ph == 'C1':
                if ph == phases[-1] and dbg is None:
                    phase_C(1, early_out=lambda co, tt: out_toks.append(
                        S.dma('sp', yv[:, co, tt * TT:(tt + 1) * TT], h[:, co, tt * TT:(tt + 1) * TT])))
                    stored[0] = True
                else:
                    phase_C(1)
            elif ph == 'D':
                phase_D()

        toks = list(out_toks)
        if not stored[0]:
            for c in range(NC8):
                toks.append(S.dma('sp' if c % 2 == 0 else 'act', yv[:, c, :], h[:, c, :]))
        for t in toks + dbg_toks:
            S.wait_tok('sp', t)
        build_program.stats = (S.nops, S.nwaits)
    return nc


_CACHE = {}


LAST = {}


def run(inputs, phases=('A', 'B0', 'C0', 'D', 'B1', 'C1'), debug=False):
    key = (tuple(phases), debug)
    if key not in _CACHE:
        _CACHE[key] = build_program(phases, debug)
    nc = _CACHE[key]
    x = np.asarray(inputs['x'], dtype=np.float32)
    p = np.asarray(inputs['p'], dtype=np.float32)
    prm = pack_params(inputs)
    cst = make_consts()
    shared = {
        "prm": prm, "cst": cst,
        "w_pw1": np.ascontiguousarray(np.asarray(inputs['conv_w_pw1'], dtype=np.float32)[0]),
        "w_pw2": np.ascontiguousarray(np.asarray(inputs['conv_w_pw2'], dtype=np.float32)[0]),
        "w_qkv": np.ascontiguousarray(np.asarray(inputs['attn_w_qkv'], dtype=np.float32)[0]),
        "w_o": np.ascontiguousarray(np.asarray(inputs['attn_w_o'], dtype=np.float32)[0]),
        "w_gate": np.ascontiguousarray(np.asarray(inputs['moe_w_gate'], dtype=np.float32)),
        "w_up": np.ascontiguousarray(np.asarray(inputs['moe_w_up'], dtype=np.float32)),
        "w_down": np.ascontiguousarray(np.asarray(inputs['moe_w_down'], dtype=np.float32)),
        "w_pg": np.ascontiguousarray(np.asarray(inputs['ple_w_gate'], dtype=np.float32)),
        "w_pp": np.ascontiguousarray(np.asarray(inputs['ple_w_proj'], dtype=np.float32)),
    }
    in_maps = []
    for b in range(NCORES):
        m = dict(shared)
        m["xT"] = np.ascontiguousarray(x[b].T)
        m["pT"] = np.ascontiguousarray(p[:, b].transpose(0, 2, 1))
        in_maps.append(m)
    res = run_bass_kernel_spmd(nc, in_maps, core_ids=list(range(NCORES)))
    if debug:
        LAST['dbg'] = [np.asarray(res.results[b]["dbg"]) for b in range(NCORES)]
    out = np.stack([np.asarray(res.results[b]["yT"], dtype=np.float32).T for b in range(NCORES)], axis=0)
    return np.ascontiguousarray(out)


def kernel(**inputs):
    return run(inputs)
```

```python
from contextlib import ExitStack

import numpy as np
import concourse.bass as bass
import concourse.mybir as mybir
from concourse.bass_utils import run_bass_kernel_spmd

F32 = mybir.dt.float32
BF16 = mybir.dt.bfloat16
AF = mybir.ActivationFunctionType
ALU = mybir.AluOpType
AX = mybir.AxisListType

D = 1024
S_LEN = 2048
NC8 = 8
NTT = 4
TT = 512
HALO = 30
NH = 8
HD = 128
NE = 16
EH = 512
PLE = 256
EPS = 1e-6
NCORES = 8


class Sched:
    def __init__(self, nc, ctx):
        self.nc = nc
        self.ctx = ctx
        self.eng = {'pe': nc.tensor, 'act': nc.scalar, 'dve': nc.vector, 'pool': nc.gpsimd, 'sp': nc.sync}
        self.sem = {}
        self.cnt = {}
        self.nsem = 0
        for e in ['pe', 'act', 'dve', 'pool']:
            self._new_sem(e)
        self.seen = {e: {} for e in self.eng}
        self.W = {}
        self.R = {}
        self.pend = {e: [] for e in self.eng}
        self.dma_sems = []
        self.dma_cnt = []
        self.dma_rr = {'hw': 0, 'sw': 0}
        self.dma_pool = {'hw': list(range(0, 8)), 'sw': list(range(8, 32))}
        for i in range(32):
            self.dma_sems.append(ctx.enter_context(nc.semaphore(f"dq{i}")))
            self.dma_cnt.append(0)
        self.nwaits = 0
        self.nops = 0

    def _new_sem(self, e):
        self.nsem += 1
        self.sem[e] = self.ctx.enter_context(self.nc.semaphore(f"s_{e}_{self.nsem}"))
        self.cnt[e] = 0

    @staticmethod
    def regions(ap):
        t = ap.tensor
        if str(ap.space) == 'DRAM':
            return []
        shape = t.shape
        row = 1
        for s in shape[1:]:
            row *= s
        off = int(ap.offset)
        dims = ap.ap
        p0 = off // row
        f0 = off % row
        npart = dims[0][1]
        fd = sorted([(abs(s), n) for (s, n) in dims[1:] if n > 1 and s != 0], reverse=True)

        def expand(ds, base, budget):
            if not ds:
                return [(base, base + 1)]
            s0, n0 = ds[0]
            inner = 1
            for (t_, m_) in ds[1:]:
                inner += (m_ - 1) * t_
            if s0 > inner and n0 <= budget:
                out = []
                for i in range(n0):
                    out += expand(ds[1:], base + i * s0, budget // n0)
                return out
            return [(base, base + inner + (n0 - 1) * s0)]

        name = t.name
        return [(name, p0, p0 + npart, a, b) for (a, b) in expand(fd, f0, 32)]

    @staticmethod
    def _ov(a, r):
        return a[0] < r[2] and r[1] < a[1] and a[2] < r[4] and r[3] < a[3]

    def _collect(self, engine, ins, outs):
        toks = []
        rin = [r for a in ins for r in self.regions(a)]
        rout = [r for a in outs for r in self.regions(a)]
        same = engine == 'pe'
        for r in rin:
            if r is None:
                continue
            for w in self.W.get(r[0], ()):
                if self._ov(w, r):
                    toks.append(w[4])
        for r in rout:
            if r is None:
                continue
            for w in self.W.get(r[0], ()):
                if self._ov(w, r) and not (same and w[4][2] == engine):
                    toks.append(w[4])
            for rd in self.R.get(r[0], ()):
                if self._ov(rd, r) and not (same and rd[4][2] == engine):
                    toks.append(rd[4])
        return toks, rin, rout

    def _wait(self, engine, toks):
        need = {}
        for (sem, val, src) in toks:
            k = id(sem)
            if k not in need or need[k][1] < val:
                need[k] = (sem, val, src)
        for k, (sem, val, src) in need.items():
            if src == engine and engine == 'pe':
                continue
            if self.seen[engine].get(k, 0) < val:
                self.eng[engine].wait_ge(sem, val)
                self.seen[engine][k] = val
                self.nwaits += 1

    def _record(self, tok, rin, rout):
        for r in rin:
            if r is None:
                continue
            lst = self.R.setdefault(r[0], [])
            lst[:] = [x for x in lst if not (x[4][0] is tok[0] and x[0] >= r[1] and x[1] <= r[2]
                                             and x[2] >= r[3] and x[3] <= r[4])]
            lst.append((r[1], r[2], r[3], r[4], tok))
        for r in rout:
            if r is None:
                continue
            lst = self.W.setdefault(r[0], [])
            lst[:] = [x for x in lst if not (x[0] >= r[1] and x[1] <= r[2] and x[2] >= r[3] and x[3] <= r[4])]
            lst.append((r[1], r[2], r[3], r[4], tok))
            rl = self.R.get(r[0])
            if rl:
                rl[:] = [x for x in rl if not (x[0] >= r[1] and x[1] <= r[2] and x[2] >= r[3] and x[3] <= r[4])]

    def op(self, engine, fn, ins=(), outs=(), inc=True):
        toks, rin, rout = self._collect(engine, ins, outs)
        self._wait(engine, toks)
        ins_obj = fn(self.eng[engine])
        self.nops += 1
        if inc:
            if self.cnt[engine] >= 30000:
                self._new_sem(engine)
            self.cnt[engine] += 1
            ins_obj.then_inc(self.sem[engine], 1)
            tok = (self.sem[engine], self.cnt[engine], engine)
            for (pi, po) in self.pend[engine]:
                self._record(tok, pi, po)
            self.pend[engine] = []
            self._record(tok, rin, rout)
            return tok
        else:
            self.pend[engine].append((rin, rout))
            return None

    def dma(self, queue, out, in_, **kw):
        kind = 'sw' if queue == 'pool' else 'hw'
        lst = self.dma_pool[kind]
        i = lst[self.dma_rr[kind] % len(lst)]
        self.dma_rr[kind] += 1
        sem = self.dma_sems[i]
        toks, rin, rout = self._collect('dma', [in_], [out])
        if self.dma_cnt[i] > 0:
            toks.append((sem, self.dma_cnt[i], 'dma'))
        self._wait(queue, toks)
        ins_obj = self.eng[queue].dma_start(out=out, in_=in_, **kw)
        self.dma_cnt[i] += 16
        ins_obj.then_inc(sem, 16)
        tok = (sem, self.dma_cnt[i], 'dma')
        self._record(tok, rin, rout)
        return tok

    def wait_tok(self, engine, tok):
        self._wait(engine, [tok])

    def mm(self, out, lhsT, rhs, start, stop, inc=None, **kw):
        inc = True
        return self.op('pe', lambda e: e.matmul(out, lhsT=lhsT, rhs=rhs, start=start, stop=stop, **kw),
                       ins=[lhsT, rhs], outs=[out], inc=inc)

    def transpose(self, out, in_, ident):
        return self.op('pe', lambda e: e.transpose(out, in_, ident), ins=[in_, ident], outs=[out])

    def act(self, out, in_, func, bias=None, scale=None, eng='act'):
        ins = [in_]
        kw = {}
        if bias is not None:
            kw['bias'] = bias
            if not isinstance(bias, (int, float)):
                ins.append(bias)
        if scale is not None:
            kw['scale'] = scale
            if not isinstance(scale, (int, float)):
                ins.append(scale)
        return self.op('act', lambda e: e.activation(out=out, in_=in_, func=func, **kw), ins=ins, outs=[out])

    def tt(self, eng, out, in0, in1, op):
        return self.op(eng, lambda e: e.tensor_tensor(out=out, in0=in0, in1=in1, op=op), ins=[in0, in1], outs=[out])

    def ts(self, eng, out, in0, s1, op0, s2=None, op1=None):
        ins = [in0]
        if not isinstance(s1, (int, float)):
            ins.append(s1)
        if s2 is not None and not isinstance(s2, (int, float)):
            ins.append(s2)
        if op1 is None:
            return self.op(eng, lambda e: e.tensor_scalar(out=out, in0=in0, scalar1=s1, scalar2=None, op0=op0),
                           ins=ins, outs=[out])
        return self.op(eng, lambda e: e.tensor_scalar(out=out, in0=in0, scalar1=s1, scalar2=s2, op0=op0, op1=op1),
                       ins=ins, outs=[out])

    def stt(self, out, in0, scalar, in1, op0, op1):
        ins = [in0, in1]
        if not isinstance(scalar, (int, float)):
            ins.append(scalar)
        return self.op('dve', lambda e: e.scalar_tensor_tensor(out=out, in0=in0, scalar=scalar, in1=in1, op0=op0, op1=op1),
                       ins=ins, outs=[out])

    def copy(self, eng, out, in_):
        if eng == 'act':
            return self.op('act', lambda e: e.copy(out=out, in_=in_), ins=[in_], outs=[out])
        return self.op(eng, lambda e: e.tensor_copy(out=out, in_=in_), ins=[in_], outs=[out])

    def recip(self, out, in_):
        return self.op('dve', lambda e: e.reciprocal(out=out, in_=in_), ins=[in_], outs=[out])

    def reduce(self, out, in_, op):
        return self.op('dve', lambda e: e.tensor_reduce(out=out, in_=in_, axis=AX.X, op=op), ins=[in_], outs=[out])

    def memset(self, eng, ap, val):
        return self.op(eng, lambda e: e.memset(ap, val), ins=[], outs=[ap])


class Packer:
    def __init__(self):
        self.cols = []
        self.off = {}
        self.n = 0

    def add(self, name, arr):
        arr = np.ascontiguousarray(arr, dtype=np.float32)
        assert arr.shape[0] == 128, (name, arr.shape)
        arr = arr.reshape(128, -1)
        self.off[name] = self.n
        self.n += arr.shape[1]
        self.cols.append(arr)

    def build(self):
        return np.ascontiguousarray(np.concatenate(self.cols, axis=1))


def feat_cols(v):
    v = np.asarray(v, dtype=np.float32)
    return v.reshape(-1, 128).T


def prm_layout():
    off = {}
    n = 0
    for name, w in [('g_mix', 16), ('g_ffn', 16), ('g_ple', 16), ('b_pw1', 16), ('w_dw', 248), ('b_dw', 8),
                    ('ln_g', 8), ('ln_b', 8), ('b_pw2', 8), ('q_gain', 1), ('k_gain', 1),
                    ('wr0', 160), ('wr1', 160), ('rb0', 20), ('rb1', 20)]:
        off[name] = n
        n += w
    return off, n


def pack_params(inp):
    pk = Packer()
    pk.add('g_mix', feat_cols(inp['g_mix']))
    pk.add('g_ffn', feat_cols(inp['g_ffn']))
    pk.add('g_ple', feat_cols(inp['g_ple']))
    pk.add('b_pw1', feat_cols(inp['conv_b_pw1'][0]))
    wdw = np.asarray(inp['conv_w_dw'][0], dtype=np.float32)
    pk.add('w_dw', wdw.reshape(31, 8, 128).transpose(2, 1, 0).reshape(128, 248))
    pk.add('b_dw', feat_cols(inp['conv_b_dw'][0]))
    pk.add('ln_g', feat_cols(inp['conv_ln_g'][0]))
    pk.add('ln_b', feat_cols(inp['conv_ln_b'][0]))
    pk.add('b_pw2', feat_cols(inp['conv_b_pw2'][0]))
    pk.add('q_gain', np.asarray(inp['attn_q_gain'][0], dtype=np.float32).reshape(128, 1))
    pk.add('k_gain', np.asarray(inp['attn_k_gain'][0], dtype=np.float32).reshape(128, 1))
    for l in range(2):
        wg = np.asarray(inp['moe_w_group'][l], dtype=np.float32)
        wr = np.asarray(inp['moe_w_router'][l], dtype=np.float32)
        wall = np.concatenate([wg, wr.transpose(1, 0, 2).reshape(1024, 16)], axis=1)
        pk.add(f'wr{l}', wall.reshape(8, 128, 20).transpose(1, 0, 2).reshape(128, 160))
    for l in range(2):
        b = np.concatenate([np.asarray(inp['moe_b_group'][l], dtype=np.float32).reshape(4),
                            np.asarray(inp['moe_b_router'][l], dtype=np.float32).reshape(16)])
        pk.add(f'rb{l}', np.tile(b[None, :], (128, 1)))
    arr = pk.build()
    off, n = prm_layout()
    assert off == pk.off and n == arr.shape[1]
    return arr


def cst_layout():
    off = {}
    n = 0
    for name, w in [('ident', 128), ('tri', 128), ('ab', 8 * 17), ('alc', 16), ('negm', 64), ('esel', 9 * 128)]:
        off[name] = n
        n += w
    return off, n


def make_consts():
    pk = Packer()
    pk.add('ident', np.eye(128, dtype=np.float32))
    p = np.arange(128)[:, None]
    f = np.arange(128)[None, :]
    pk.add('tri', (f >= p).astype(np.float32))
    slopes = np.array([2.0 ** (-(i + 1)) for i in range(8)], dtype=np.float64)
    ab = np.zeros((128, 8, 17), dtype=np.float64)
    for h in range(8):
        for d in range(17):
            ab[:, h, d] = slopes[h] * (np.arange(128) - 128.0 * (d - 1))
    pk.add('ab', ab.reshape(128, 136))
    alc = np.zeros((128, 8, 2), dtype=np.float64)
    for h in range(8):
        for par in range(2):
            alc[:, h, par] = -slopes[h] * (par * 128 + np.arange(128))
    pk.add('alc', alc.reshape(128, 16))
    negm = np.zeros((128, 8, 8), dtype=np.float32)
    for i8 in range(8):
        j = (8 + i8) // 2
        for n in range(8):
            if n >= j:
                negm[:, i8, n] = -1e30
    pk.add('negm', negm.reshape(128, 64))
    esel = np.zeros((128, 9, 128), dtype=np.float32)
    for s in range(9):
        esel[0, s, :] = 1.0
        if s < 8:
            esel[32 + s, s, :] = 1.0
    pk.add('esel', esel.reshape(128, 9 * 128))
    arr = pk.build()
    off, n = cst_layout()
    assert off == pk.off and n == arr.shape[1]
    return arr


def build_program(phases=('A', 'B0', 'C0', 'D', 'B1', 'C1'), debug=False):
    nc = bass.Bass("TRN2", target_bir_lowering=False)
    PO, NPRM = prm_layout()
    CO, NCST = cst_layout()

    def din(name, shape):
        return nc.dram_tensor(name, list(shape), F32, kind="ExternalInput").ap()

    xT = din("xT", [D, S_LEN])
    pT = din("pT", [2, PLE, S_LEN])
    prm_d = din("prm", [128, NPRM])
    cst_d = din("cst", [128, NCST])
    w_pw1 = din("w_pw1", [D, 2 * D])
    w_pw2 = din("w_pw2", [D, D])
    w_qkv = din("w_qkv", [D, 3 * D])
    w_o = din("w_o", [D, D])
    w_gate = din("w_gate", [2, NE, D, EH])
    w_up = din("w_up", [2, NE, D, EH])
    w_down = din("w_down", [2, NE, EH, D])
    w_pg = din("w_pg", [2, D, D])
    w_pp = din("w_pp", [2, PLE, D])
    yT = nc.dram_tensor("yT", [D, S_LEN], F32, kind="ExternalOutput").ap()
    dbg = nc.dram_tensor("dbg", [128, 8192], F32, kind="ExternalOutput").ap() if debug else None

    with ExitStack() as ctx:
        S = Sched(nc, ctx)

        def sb(name, shape, dt, c=ctx):
            return c.enter_context(nc.sbuf_tensor(name, list(shape), dt))

        h = sb("h", [128, NC8, S_LEN], F32)
        xg = sb("xg", [128, NC8, HALO + S_LEN], BF16)
        prm = sb("prm_s", [128, NPRM], F32)
        cst = sb("cst_s", [128, NCST], F32)
        ones_bf = sb("ones_bf", [128, 128], BF16)
        tri_bf = sb("tri_bf", [128, 128], BF16)
        esel_bf = sb("esel_bf", [128, 9, 128], BF16)
        sq = sb("sq", [128, 2, TT], BF16)
        rt = sb("rt", [128, TT], F32)
        rstd = sb("rstd", [128, TT], F32)
        PS = [ctx.enter_context(nc.psum_tensor(f"ps{i}", [128, 512], F32)) for i in range(8)]
        ps_rr = [0]
        ps_set = [list(range(8))]

        def next_ps():
            lst = ps_set[0]
            b = lst[ps_rr[0] % len(lst)]
            ps_rr[0] += 1
            return PS[b]

        ident = cst[:, CO['ident']:CO['ident'] + 128]

        def xn(c, lo, n):
            return xg[:, c, HALO + lo:HALO + lo + n]

        def P(name, j=0, n=1):
            return prm[:, PO[name] + j:PO[name] + j + n]

        S.dma('sp', cst[:], cst_d)
        S.dma('sp', prm[:], prm_d)
        xv = xT.rearrange("(c p) t -> p c t", p=128)
        for tt0 in range(NTT):
            for c in range(NC8):
                S.dma('sp' if c % 2 == 0 else 'act', h[:, c, tt0 * TT:(tt0 + 1) * TT], xv[:, c, tt0 * TT:(tt0 + 1) * TT])
        S.memset('dve', ones_bf[:], 1.0)
        S.copy('dve', tri_bf[:], cst[:, CO['tri']:CO['tri'] + 128])
        S.copy('dve', esel_bf[:].rearrange("p a b -> p (a b)"), cst[:, CO['esel']:CO['esel'] + 9 * 128])
        S.memset('pool', xg[:, :, 0:HALO], 0.0)

        def tts(tt):
            return slice(tt * TT, (tt + 1) * TT)

        dbg_toks = []

        def dump(ap, col):
            if dbg is None:
                return
            np_, n = ap.shape[0], ap.shape[1]
            dbg_toks.append(S.dma('sp', dbg[0:np_, col:col + n], ap))

        def rms_tile(tt, gname, gl, out_fn):
            ps = next_ps()
            for c in range(NC8):
                S.act(sq[:, c % 2, :], h[:, c, tts(tt)], AF.Square)
                S.mm(ps[:], ones_bf[:], sq[:, c % 2, :], start=(c == 0), stop=(c == NC8 - 1))
            S.act(rt[:], ps[:], AF.Ln, bias=EPS, scale=1.0 / D)
            S.act(rstd[:], rt[:], AF.Exp, scale=-0.5)
            for c in range(NC8):
                S.stt(out_fn(c), h[:, c, tts(tt)], P(gname, gl * 8 + c), rstd[:], ALU.mult, ALU.mult)

        def barrier():
            toks = []
            for e in ['pe', 'act', 'dve', 'pool']:
                if S.cnt[e] > 0:
                    toks.append((S.sem[e], S.cnt[e], e))
            for i, sem in enumerate(S.dma_sems):
                if S.dma_cnt[i] > 0:
                    toks.append((sem, S.dma_cnt[i], 'dma'))
            for e in ['pe', 'act', 'dve', 'pool', 'sp']:
                S._wait(e, toks)

        def phase_A():
            with ExitStack() as pc:
                rms_tile(0, 'g_mix', 0, lambda c: xn(c, 0, TT))
                glu = sb("glu", [128, NC8, HALO + S_LEN], BF16, pc)
                S.memset('pool', glu[:, :, 0:HALO], 0.0)
                with ExitStack() as p2:
                    w1 = sb("w1", [128, NC8, 2 * D], BF16, p2)
                    sg = sb("sgA", [128, 2, TT], F32, p2)
                    w1v = w_pw1.rearrange("(c p) n -> p c n", p=128)
                    for c in range(NC8):
                        S.dma('pool', w1[:, c, :], w1v[:, c, :])
                    for tt in range(NTT):
                        if tt + 1 < NTT:
                            rms_tile(tt + 1, 'g_mix', 0, lambda c, t1=tt + 1: xn(c, t1 * TT, TT))
                        for c in range(NC8):
                            pa = next_ps()
                            pg = next_ps()
                            for k in range(NC8):
                                S.mm(pa[:], w1[:, k, c * 128:(c + 1) * 128], xn(k, tt * TT, TT), k == 0, k == NC8 - 1)
                            for k in range(NC8):
                                S.mm(pg[:], w1[:, k, D + c * 128:D + (c + 1) * 128], xn(k, tt * TT, TT), k == 0, k == NC8 - 1)
                            S.act(sg[:, c % 2, :], pg[:], AF.Sigmoid, bias=P('b_pw1', 8 + c))
                            S.stt(glu[:, c, HALO + tt * TT:HALO + (tt + 1) * TT], pa[:], P('b_pw1', c), sg[:, c % 2, :],
                                  ALU.add, ALU.mult)
                    barrier()
                dg = sb("dg", [128, 2, 31, 128], BF16, pc)
                w2 = sb("w2", [128, NC8, D], BF16, pc)
                w2v = w_pw2.rearrange("(c p) n -> p c n", p=128)
                for c in range(NC8):
                    S.dma('pool', w2[:, c, :], w2v[:, c, :])

                def cvb(c, tt):
                    return xn(c, tt * TT, TT)

                for c in range(NC8):
                    wd = prm[:, PO['w_dw'] + c * 31:PO['w_dw'] + (c + 1) * 31]
                    S.tt('dve', dg[:, c % 2], ident.unsqueeze(1).to_broadcast([128, 31, 128]),
                         wd.unsqueeze(2).to_broadcast([128, 31, 128]), ALU.mult)
                    for tt in range(NTT):
                        ps = next_ps()
                        for j in range(31):
                            S.mm(ps[:], dg[:, c % 2, j, :], glu[:, c, tt * TT + j:tt * TT + j + TT], j == 0, j == 30)
                        S.act(cvb(c, tt), ps[:], AF.Identity, bias=P('b_dw', c))
                mean = sb("meanA", [128, TT], F32, pc)
                m2 = sb("m2A", [128, TT], F32, pc)
                var = sb("varA", [128, TT], F32, pc)
                t1 = sb("t1A", [128, 2, TT], F32, pc)
                t2 = sb("t2A", [128, 2, TT], F32, pc)
                for tt in range(NTT):
                    p1 = next_ps()
                    p2_ = next_ps()
                    for c in range(NC8):
                        S.act(sq[:, c % 2, :], cvb(c, tt), AF.Square)
                        S.mm(p2_[:], ones_bf[:], sq[:, c % 2, :], c == 0, c == NC8 - 1)
                    for c in range(NC8):
                        S.mm(p1[:], ones_bf[:], cvb(c, tt), c == 0, c == NC8 - 1)
                    S.ts('dve', mean[:], p1[:], 1.0 / D, ALU.mult)
                    S.tt('dve', m2[:], mean[:], mean[:], ALU.mult)
                    S.stt(var[:], p2_[:], 1.0 / D, m2[:], ALU.mult, ALU.subtract)
                    S.act(rt[:], var[:], AF.Ln, bias=EPS)
                    S.act(rstd[:], rt[:], AF.Exp, scale=-0.5)
                    for c in range(NC8):
                        S.tt('pool', t1[:, c % 2, :], cvb(c, tt), mean[:], ALU.subtract)
                        S.tt('dve', t2[:, c % 2, :], t1[:, c % 2, :], rstd[:], ALU.mult)
                        S.act(xn(c, tt * TT, TT), t2[:, c % 2, :], AF.Silu, bias=P('ln_b', c), scale=P('ln_g', c))
                for tt in range(NTT):
                    for co in range(NC8):
                        ps = next_ps()
                        for k in range(NC8):
                            S.mm(ps[:], w2[:, k, co * 128:(co + 1) * 128], xn(k, tt * TT, TT), k == 0, k == NC8 - 1)
                        S.stt(h[:, co, tts(tt)], ps[:], P('b_pw2', co), h[:, co, tts(tt)], ALU.add, ALU.add)
            barrier()

        def phase_B(l):
            with ExitStack() as pc:
                cT_hi = sb(f"cThi_{l}", [128, S_LEN], BF16, pc)
                cT_lo = sb(f"cTlo_{l}", [128, S_LEN], BF16, pc)
                sel16 = sb(f"sel16_{l}", [128, 16, 128], BF16, pc)
                S.copy('dve', sel16[:], cst[:, CO['ident']:CO['ident'] + 16].unsqueeze(2).to_broadcast([128, 16, 128]))
                wr = prm[:, PO[f'wr{l}']:PO[f'wr{l}'] + 160]
                psL = PS[7]
                ps_set[0] = list(range(7))
                rc = ExitStack()
                combT = sb(f"combT_{l}", [128, S_LEN], F32, rc)
                S.memset('pool', combT[:], 0.0)
                x32 = sb(f"x32_{l}", [128, NC8, TT], F32, rc)
                lb = sb(f"lb_{l}", [128, 16, 20], F32, rc)
                lT = sb(f"lT_{l}", [128, 2, TT], F32, rc)
                for tt in range(NTT):
                    rms_tile(tt, 'g_ffn', l, lambda c: x32[:, c, :])
                    for c in range(NC8):
                        S.copy('act', xn(c, tt * TT, TT), x32[:, c, :])
                    pT_ = next_ps()
                    for c in range(NC8):
                        S.mm(pT_[0:20, :], wr[:, c * 20:(c + 1) * 20], x32[:, c, :], c == 0, c == NC8 - 1)
                    S.copy('act', lT[0:20, tt % 2, :], pT_[0:20, :])
                    for i4 in range(4):
                        i = tt * 4 + i4
                        S.transpose(psL[:, i * 20:(i + 1) * 20], lT[0:20, tt % 2, i4 * 128:(i4 + 1) * 128],
                                    cst[0:20, CO['ident']:CO['ident'] + 20])
                with rc:
                    def T(name, shape):
                        return sb(f"{name}_{l}", shape, F32, rc)
                    rb = prm[:, PO[f'rb{l}']:PO[f'rb{l}'] + 20]
                    S.tt('dve', lb[:], psL[:, 0:320].rearrange("p (t n) -> p t n", n=20),
                         rb.unsqueeze(1).to_broadcast([128, 16, 20]), ALU.add)
                    lg = lb[:, :, 0:4]
                    le = lb[:, :, 4:20].rearrange("p t (g e) -> p t g e", e=4)
                    m = T("r_m", [128, 16])
                    S.reduce(m[:], lg, ALU.max)
                    dd = T("r_d", [128, 16, 4])
                    S.tt('dve', dd[:], lg, m[:].unsqueeze(2).to_broadcast([128, 16, 4]), ALU.subtract)
                    oh = T("r_oh", [128, 16, 4])
                    S.ts('dve', oh[:], dd[:], 0.0, ALU.is_equal)
                    eg = T("r_eg", [128, 16, 4])
                    S.act(eg[:], dd[:], AF.Exp)
                    se = T("r_se", [128, 16])
                    S.reduce(se[:], eg[:], ALU.add)
                    gw = T("r_gw", [128, 16])
                    S.recip(gw[:], se[:])
                    tmp = T("r_tmp", [128, 16, 4, 4])
                    S.tt('dve', tmp[:], le, oh[:].unsqueeze(3).to_broadcast([128, 16, 4, 4]), ALU.mult)
                    sel = T("r_sel", [128, 16, 4])
                    S.reduce(sel[:], tmp[:].rearrange("p t g e -> p t e g"), ALU.add)
                    m1 = T("r_m1", [128, 16])
                    S.reduce(m1[:], sel[:], ALU.max)
                    eq1 = T("r_eq1", [128, 16, 4])
                    S.tt('dve', eq1[:], sel[:], m1[:].unsqueeze(2).to_broadcast([128, 16, 4]), ALU.is_equal)
                    sel2 = T("r_sel2", [128, 16, 4])
                    S.stt(sel2[:].rearrange("p t e -> p (t e)"), eq1[:].rearrange("p t e -> p (t e)"), -1e30,
                          sel[:].rearrange("p t e -> p (t e)"), ALU.mult, ALU.add)
                    m2_ = T("r_m2", [128, 16])
                    S.reduce(m2_[:], sel2[:], ALU.max)
                    eq2 = T("r_eq2", [128, 16, 4])
                    S.tt('dve', eq2[:], sel2[:], m2_[:].unsqueeze(2).to_broadcast([128, 16, 4]), ALU.is_equal)
                    d21 = T("r_d21", [128, 16])
                    S.tt('dve', d21[:], m2_[:], m1[:], ALU.subtract)
                    e21 = T("r_e21", [128, 16])
                    S.act(e21[:], d21[:], AF.Exp)
                    den = T("r_den", [128, 16])
                    S.ts('dve', den[:], e21[:], 1.0, ALU.add)
                    w1_ = T("r_w1", [128, 16])
                    S.recip(w1_[:], den[:])
                    w2_ = T("r_w2", [128, 16])
                    S.tt('dve', w2_[:], e21[:], w1_[:], ALU.mult)
                    w1g = T("r_w1g", [128, 16])
                    S.tt('dve', w1g[:], w1_[:], gw[:], ALU.mult)
                    w2g = T("r_w2g", [128, 16])
                    S.tt('dve', w2g[:], w2_[:], gw[:], ALU.mult)
                    ea = T("r_ea", [128, 16, 4])
                    S.tt('dve', ea[:], eq1[:], w1g[:].unsqueeze(2).to_broadcast([128, 16, 4]), ALU.mult)
                    eb = T("r_eb", [128, 16, 4])
                    S.tt('dve', eb[:], eq2[:], w2g[:].unsqueeze(2).to_broadcast([128, 16, 4]), ALU.mult)
                    ew = T("r_ew", [128, 16, 4])
                    S.tt('dve', ew[:], ea[:], eb[:], ALU.add)
                    comb = T("r_comb", [128, 16, 4, 4])
                    S.tt('dve', comb[:], oh[:].unsqueeze(3).to_broadcast([128, 16, 4, 4]),
                         ew[:].unsqueeze(2).to_broadcast([128, 16, 4, 4]), ALU.mult)
                    for g4 in range(4):
                        ps = next_ps()
                        for i4 in range(4):
                            i = g4 * 4 + i4
                            S.transpose(ps[0:16, i4 * 128:(i4 + 1) * 128],
                                        comb[:, i, :, :].rearrange("p g e -> p (g e)"), ident)
                        S.copy('act', combT[0:16, g4 * 512:(g4 + 1) * 512], ps[0:16, :])
                    S.copy('dve', cT_hi[:], combT[:])
                    S.tt('dve', combT[:], combT[:], cT_hi[:], ALU.subtract)
                    S.copy('dve', cT_lo[:], combT[:])
                    barrier()
                ps_set[0] = list(range(8))
                wg = sb(f"wg_{l}", [128, 2, NC8, EH], BF16, pc)
                wu = sb(f"wu_{l}", [128, 2, NC8, EH], BF16, pc)
                wd = sb(f"wd_{l}", [128, 2, 4, D], BF16, pc)
                sgs = sb(f"sgs_{l}", [128, 3, TT], F32, pc)
                tus = sb(f"tus_{l}", [128, 3, TT], F32, pc)
                hdn = sb(f"hdn_{l}", [128, 2, 4, TT], BF16, pc)

                def load_w(e):
                    b = e % 2
                    S.dma('pool', wg[:, b], w_gate[l, e].rearrange("(c p) n -> p c n", p=128))
                    S.dma('pool', wu[:, b], w_up[l, e].rearrange("(c p) n -> p c n", p=128))
                    S.dma('pool', wd[:, b], w_down[l, e].rearrange("(c p) n -> p c n", p=128))

                units = [(e, tt) for e in range(NE) for tt in range(NTT)]
                rr = [0]

                def gu(u, ui):
                    e, tt = u
                    b = e % 2
                    pcb = PS[6 + ui % 2]
                    S.mm(pcb[:], sel16[:, e, :], cT_hi[:, tts(tt)], True, False)
                    S.mm(pcb[:], sel16[:, e, :], cT_lo[:, tts(tt)], False, True)
                    hb = hdn[:, ui % 2]
                    for hc in range(4):
                        pg = next_ps()
                        pu = next_ps()
                        for k in range(NC8):
                            S.mm(pg[:], wg[:, b, k, hc * 128:(hc + 1) * 128], xn(k, tt * TT, TT), k == 0, k == NC8 - 1)
                        for k in range(NC8):
                            S.mm(pu[:], wu[:, b, k, hc * 128:(hc + 1) * 128], xn(k, tt * TT, TT), k == 0, k == NC8 - 1)
                        r = rr[0] % 3
                        rr[0] += 1
                        S.act(sgs[:, r, :], pg[:], AF.Silu)
                        S.tt('dve', tus[:, r, :], pu[:], sgs[:, r, :], ALU.mult)
                        S.tt('dve', hb[:, hc, :], tus[:, r, :], pcb[:], ALU.mult)

                def down(u, ui):
                    e, tt = u
                    b = e % 2
                    hb = hdn[:, ui % 2]
                    for dc in range(NC8):
                        pd = next_ps()
                        for hc in range(4):
                            S.mm(pd[:], wd[:, b, hc, dc * 128:(dc + 1) * 128], hb[:, hc, :], hc == 0, hc == 3)
                        S.tt('dve', h[:, dc, tts(tt)], pd[:], h[:, dc, tts(tt)], ALU.add)

                load_w(0)
                ps_set[0] = list(range(6))
                for ui, u in enumerate(units):
                    e, tt = u
                    if tt == 0 and e + 1 < NE:
                        pass
                    gu(u, ui)
                    if ui > 0:
                        down(units[ui - 1], ui - 1)
                    if tt == 0 and e + 1 < NE:
                        load_w(e + 1)
                down(units[-1], len(units) - 1)
                ps_set[0] = list(range(8))
            barrier()

        def phase_C(l, early_out=None):
            with ExitStack() as pc:
                wpg = sb(f"wpg_{l}", [128, NC8, D], BF16, pc)
                wpp = sb(f"wpp_{l}", [128, 2, D], BF16, pc)
                ptb = sb(f"ptb_{l}", [128, 2, S_LEN], BF16, pc)
                sgc = sb(f"sgc_{l}", [128, 3, TT], F32, pc)
                tc_ = sb(f"tc_{l}", [128, 3, TT], F32, pc)
                wv = w_pg[l].rearrange("(c p) n -> p c n", p=128)
                for c in range(NC8):
                    S.dma('pool', wpg[:, c, :], wv[:, c, :])
                S.dma('pool', wpp[:], w_pp[l].rearrange("(c p) n -> p c n", p=128))
                pv = pT[l].rearrange("(c p) t -> p c t", p=128)
                for c in range(2):
                    S.dma('pool', ptb[:, c, :], pv[:, c, :])
                rms_tile(0, 'g_ple', l, lambda c: xn(c, 0, TT))
                r = 0
                for tt in range(NTT):
                    if tt + 1 < NTT:
                        rms_tile(tt + 1, 'g_ple', l, lambda c, t1=tt + 1: xn(c, t1 * TT, TT))
                    for co in range(NC8):
                        pg = next_ps()
                        pp = next_ps()
                        for k in range(NC8):
                            S.mm(pg[:], wpg[:, k, co * 128:(co + 1) * 128], xn(k, tt * TT, TT), k == 0, k == NC8 - 1)
                        for k in range(2):
                            S.mm(pp[:], wpp[:, k, co * 128:(co + 1) * 128], ptb[:, k, tts(tt)], k == 0, k == 1)
                        S.act(sgc[:, r % 3, :], pg[:], AF.Sigmoid)
                        S.tt('dve', tc_[:, r % 3, :], pp[:], sgc[:, r % 3, :], ALU.mult)
                        S.tt('pool', h[:, co, tts(tt)], h[:, co, tts(tt)], tc_[:, r % 3, :], ALU.add)
                        if early_out is not None:
                            early_out(co, tt)
                        r += 1
            barrier()

        def phase_D():
            SCALE = HD ** -0.5
            with ExitStack() as pc:
                ao = sb("ao", [128, NH, S_LEN], BF16, pc)
                wh = sb("wh", [128, 2, 3, NC8, HD], BF16, pc)
                qT_ = sb("qT", [128, S_LEN], BF16, pc)
                kT_ = sb("kT", [128, S_LEN], BF16, pc)
                vh = sb("vh", [128, 16, HD], BF16, pc)
                q32 = sb("q32", [128, 2, TT], F32, pc)
                ksum = sb("ksum", [128, 8], F32, pc)
                gs = sb("gs", [128, 8, 8], F32, pc)
                top8 = sb("top8", [128, 8, 8], F32, pc)
                ext = sb("ext", [128, 16, 40], F32, pc)
                rh = sb("rh", [128, S_LEN], BF16, pc)
                rtD1 = sb("rtD1", [128, TT], F32, pc)
                rsD1 = sb("rsD1", [128, TT], F32, pc)
                rtD = [rt, rtD1]
                rsD = [rstd, rsD1]
                pt_ = sb("pt", [128, 7, 256], BF16, pc)
                rden = sb("rden", [128, 2, 256], F32, pc)
                wo = sb("wo", [128, NC8, D], BF16, pc)
                rms_tile(0, 'g_mix', 1, lambda c: xn(c, 0, TT))
                rms_tile(1, 'g_mix', 1, lambda c: xn(c, TT, TT))
                rms_pending = [2, 3]
                S.memset('pool', ext[:], 0.0)
                S.memset('pool', rh[:], 0.0)
                wqv = w_qkv.rearrange("(c p) n -> p c n", p=128)

                def load_head(hh):
                    b = hh % 2
                    for j in range(3):
                        S.dma('pool', wh[:, b, j], wqv[:, :, j * D + hh * HD:j * D + (hh + 1) * HD])

                load_head(0)
                wov = w_o.rearrange("(c p) n -> p c n", p=128)
                for c in range(NC8):
                    S.dma('pool', wo[:, c, :], wov[:, c, :])
                gen = [0, 1, 2, 3, 6, 7]
                for hh in range(NH):
                    b = hh % 2
                    if hh + 1 < NH:
                        load_head(hh + 1)
                    ps_set[0] = [0, 1, 2, 4, 5, 6, 7]
                    psg = PS[3]
                    tiles = [(1, tt) for tt in range(NTT)] + [(0, tt) for tt in range(NTT)]
                    pst = {}

                    def stage1(n):
                        j, tt = tiles[n]
                        ps = next_ps()
                        for k in range(NC8):
                            S.mm(ps[:], wh[:, b, j, k, :], xn(k, tt * TT, TT), k == 0, k == NC8 - 1)
                        S.act(sq[:, n % 2, :], ps[:], AF.Square)
                        p2_ = next_ps()
                        S.mm(p2_[:], ones_bf[:], sq[:, n % 2, :], True, True)
                        S.act(rtD[n % 2][:], p2_[:], AF.Ln, bias=EPS, scale=1.0 / HD)
                        S.act(rsD[n % 2][:], rtD[n % 2][:], AF.Exp, scale=-0.5)
                        pst[n] = ps

                    def stage2(n):
                        j, tt = tiles[n]
                        ps = pst[n]
                        d32 = q32[:, n % 2, :]
                        if j == 1:
                            S.stt(d32, ps[:], P('k_gain'), rsD[n % 2][:], ALU.mult, ALU.mult)
                            S.copy('act', kT_[:, tts(tt)], d32)
                            S.reduce(ksum[:, 2 * tt:2 * tt + 2], d32.rearrange("p (a b) -> p a b", b=256), ALU.add)
                        else:
                            S.stt(d32, ps[:], P('q_gain'), rsD[n % 2][:], ALU.mult, ALU.mult)
                            S.act(qT_[:, tts(tt)], d32, AF.Copy, scale=SCALE)
                            if tt >= 2:
                                for i4 in range(4):
                                    i8 = tt * 4 + i4 - 8
                                    S.mm(psg[:, i8 * 8:(i8 + 1) * 8], d32[:, i4 * 128:(i4 + 1) * 128], ksum[:], True, True)

                    stage1(0)
                    for n in range(len(tiles)):
                        if n + 1 < len(tiles):
                            stage1(n + 1)
                        stage2(n)
                        while n == 0 and rms_pending:
                            t1 = rms_pending.pop(0)
                            rms_tile(t1, 'g_mix', 1, lambda c, t1=t1: xn(c, t1 * TT, TT))
                    ps_set[0] = gen
                    S.tt('dve', gs[:], psg[:, 0:64].rearrange("p (a b) -> p a b", b=8),
                         cst[:, CO['negm']:CO['negm'] + 64].rearrange("p (a b) -> p a b", b=8), ALU.add)
                    for i8 in range(8):
                        S.op('dve', lambda e, i8=i8: e.max(out=top8[:, i8, :], in_=gs[:, i8, :]),
                             ins=[gs[:, i8, :]], outs=[top8[:, i8, :]])
                    for i8 in range(8):
                        S.ts('dve', ext[:, 8 + i8, 32:40], gs[:, i8, :], top8[:, i8, 2:3], ALU.is_lt, -30000.0, ALU.mult)
                    S.copy('dve', ext[:, :, 0].rearrange("p (a b) -> p a b", b=2),
                           cst[:, CO['alc'] + hh * 2:CO['alc'] + hh * 2 + 2].unsqueeze(1).to_broadcast([128, 8, 2]))
                    for g4 in range(4):
                        ps = next_ps()
                        for i4 in range(4):
                            i = g4 * 4 + i4
                            S.transpose(ps[0:40, i4 * 128:(i4 + 1) * 128], ext[:, i, :], ident)
                        S.copy('act', rh[0:40, g4 * 512:(g4 + 1) * 512], ps[0:40, :])
                    for g4 in range(4):
                        ps = next_ps()
                        for i4 in range(4):
                            i = g4 * 4 + i4
                            for k in range(NC8):
                                S.mm(ps[:, i4 * 128:(i4 + 1) * 128], xn(k, i * 128, 128), wh[:, b, 2, k, :],
                                     k == 0, k == NC8 - 1)
                        S.copy('act', vh[:, g4 * 4:(g4 + 1) * 4, :].rearrange("p a b -> p (a b)"), ps[:])
                    prr = [0]
                    for jq in range(8):
                        pod = PS[4 + jq % 2]
                        kts = list(range(2 * jq + 2))
                        info = []
                        for kt in kts:
                            n = kt // 2
                            own = (n == jq)
                            second = own and (kt == 2 * jq + 1)
                            c0 = 128 if second else 0
                            nq = 128 if second else 256
                            ssel = 8 if (own or jq <= 3) else n
                            info.append((kt, own, c0, nq, ssel))
                        stile = {}

                        def emit_s(idx):
                            kt, own, c0, nq, ssel = info[idx]
                            ps = next_ps()
                            q0 = jq * 256 + c0
                            S.mm(ps[:, 0:nq], kT_[:, kt * 128:(kt + 1) * 128], qT_[:, q0:q0 + nq], True, False)
                            S.mm(ps[:, 0:nq], esel_bf[:, ssel, :], rh[:, q0:q0 + nq], False, True)
                            pb = pt_[:, prr[0] % 7, :]
                            prr[0] += 1
                            dlt = 2 * jq - kt + 1
                            S.act(pb[:, 0:nq], ps[:, 0:nq], AF.Exp,
                                  bias=cst[:, CO['ab'] + hh * 17 + dlt:CO['ab'] + hh * 17 + dlt + 1])
                            if own:
                                S.tt('pool', pb[:, 0:128], pb[:, 0:128], tri_bf[:], ALU.mult)
                            stile[idx] = pb

                        def emit_pv(idx):
                            kt, own, c0, nq, ssel = info[idx]
                            pb = stile[idx]
                            first = idx == 0
                            last = idx == len(info) - 1
                            S.mm(pod[:, c0:c0 + nq], vh[:, kt, :], pb[:, 0:nq], first, last, skip_group_check=True)
                            S.mm(pod[:, 256 + c0:256 + c0 + nq], ones_bf[:], pb[:, 0:nq], False, last,
                                 skip_group_check=True)

                        LA = 5
                        for idx in range(min(LA, len(info))):
                            emit_s(idx)
                        for idx in range(len(info)):
                            if idx + LA < len(info):
                                emit_s(idx + LA)
                            emit_pv(idx)
                        S.act(rden[:, jq % 2, :], pod[:, 256:512], AF.Ln)
                        S.act(rden[:, jq % 2, :], rden[:, jq % 2, :], AF.Exp, scale=-1.0)
                        S.tt('dve', ao[:, hh, jq * 256:(jq + 1) * 256], pod[:, 0:256], rden[:, jq % 2, :], ALU.mult)
                ps_set[0] = list(range(8))
                for tt in range(NTT):
                    for co in range(NC8):
                        ps = next_ps()
                        for k in range(NC8):
                            S.mm(ps[:], wo[:, k, co * 128:(co + 1) * 128], ao[:, k, tts(tt)], k == 0, k == NC8 - 1)
                        S.tt('dve', h[:, co, tts(tt)], ps[:], h[:, co, tts(tt)], ALU.add)
            barrier()

        yv = yT.rearrange("(c p) t -> p c t", p=128)
        out_toks = []
        stored = [False]
        barrier()
        for ph in phases:
            if ph == 'A':
                phase_A()
            elif ph == 'B0':
                phase_B(0)
            elif ph == 'B1':
                phase_B(1)
            elif ph == 'C0':
                phase_C(0)
            elif ph == 'C1':
                if ph == phases[-1] and dbg is None:
                    phase_C(1, early_out=lambda co, tt: out_toks.append(
                        S.dma('sp', yv[:, co, tt * TT:(tt + 1) * TT], h[:, co, tt * TT:(tt + 1) * TT])))
                    stored[0] = True
                else:
                    phase_C(1)
            elif ph == 'D':
                phase_D()

        toks = list(out_toks)
        if not stored[0]:
            for c in range(NC8):
                toks.append(S.dma('sp' if c % 2 == 0 else 'act', yv[:, c, :], h[:, c, :]))
        for t in toks + dbg_toks:
            S.wait_tok('sp', t)
        build_program.stats = (S.nops, S.nwaits)
    return nc


_CACHE = {}


LAST = {}


def run(inputs, phases=('A', 'B0', 'C0', 'D', 'B1', 'C1'), debug=False):
    key = (tuple(phases), debug)
    if key not in _CACHE:
        _CACHE[key] = build_program(phases, debug)
    nc = _CACHE[key]
    x = np.asarray(inputs['x'], dtype=np.float32)
    p = np.asarray(inputs['p'], dtype=np.float32)
    prm = pack_params(inputs)
    cst = make_consts()
    shared = {
        "prm": prm, "cst": cst,
        "w_pw1": np.ascontiguousarray(np.asarray(inputs['conv_w_pw1'], dtype=np.float32)[0]),
        "w_pw2": np.ascontiguousarray(np.asarray(inputs['conv_w_pw2'], dtype=np.float32)[0]),
        "w_qkv": np.ascontiguousarray(np.asarray(inputs['attn_w_qkv'], dtype=np.float32)[0]),
        "w_o": np.ascontiguousarray(np.asarray(inputs['attn_w_o'], dtype=np.float32)[0]),
        "w_gate": np.ascontiguousarray(np.asarray(inputs['moe_w_gate'], dtype=np.float32)),
        "w_up": np.ascontiguousarray(np.asarray(inputs['moe_w_up'], dtype=np.float32)),
        "w_down": np.ascontiguousarray(np.asarray(inputs['moe_w_down'], dtype=np.float32)),
        "w_pg": np.ascontiguousarray(np.asarray(inputs['ple_w_gate'], dtype=np.float32)),
        "w_pp": np.ascontiguousarray(np.asarray(inputs['ple_w_proj'], dtype=np.float32)),
    }
    in_maps = []
    for b in range(NCORES):
        m = dict(shared)
        m["xT"] = np.ascontiguousarray(x[b].T)
        m["pT"] = np.ascontiguousarray(p[:, b].transpose(0, 2, 1))
        in_maps.append(m)
    res = run_bass_kernel_spmd(nc, in_maps, core_ids=list(range(NCORES)))
    if debug:
        LAST['dbg'] = [np.asarray(res.results[b]["dbg"]) for b in range(NCORES)]
    out = np.stack([np.asarray(res.results[b]["yT"], dtype=np.float32).T for b in range(NCORES)], axis=0)
    return np.ascontiguousarray(out)


def kernel(**inputs):
    return run(inputs)
```
